# Optimizing a Trainium2 kernel written in Bass

```python
import jax
import jax.numpy as jnp
from jax import lax
import numpy as np

D_MODEL = 2048
BATCH = 16
SEQ = 2048
DEPTH = 4

D_MIX = D_MODEL
D_RWKV = D_MIX // 2
D_CONV = D_MIX - D_RWKV
HEAD_DIM = 64
N_HEADS = D_RWKV // HEAD_DIM
DECAY_RANK = 64
ICLR_RANK = 64
VMIX_RANK = 32
GATE_RANK = 160
CONV_WIDTH = 31
D_FF = 4 * D_MODEL
D_PLE = 256
RMS_EPS = 1e-6
GN_EPS = 64e-5
LN_EPS = 1e-5
RWKV_COLS = 3 * D_RWKV + DECAY_RANK + ICLR_RANK + GATE_RANK
D_IN = RWKV_COLS + 2 * D_CONV

kernel_name = "hybrid_rwkv7_conformer_conv_block"


def rms_norm(x, g):
    xf = x.astype(jnp.float32)
    y = xf * lax.rsqrt(jnp.mean(xf * xf, axis=-1, keepdims=True) + RMS_EPS)
    return (y * g.astype(jnp.float32)).astype(x.dtype)


def lerp_token_shift(z, mu):
    prev = jnp.pad(z[:, :-1], ((0, 0), (1, 0), (0, 0)))
    return z + (prev - z) * mu


def wkv7_scan(r, w, k, v, a, b):
    bsz, _, h, n = r.shape

    def step(state, inp):
        r_t, w_t, k_t, v_t, a_t, b_t = inp
        sa = jnp.einsum('bhvk,bhk->bhv', state, a_t)
        state = (state * w_t[:, :, None, :]
                 + sa[..., None] * b_t[:, :, None, :]
                 + v_t[..., None] * k_t[:, :, None, :])
        y_t = jnp.einsum('bhvk,bhk->bhv', state, r_t)
        return state, y_t

    xs = tuple(jnp.swapaxes(t, 0, 1) for t in (r, w, k, v, a, b))
    s0 = jnp.zeros((bsz, h, n, n), jnp.float32)
    _, ys = lax.scan(step, s0, xs)
    return jnp.swapaxes(ys, 0, 1)


def rwkv7_time_mix(z, vmix, v_first, w0, w_decay_up, a0, w_iclr_up, w_gate_up,
                   k_k, k_a, r_k, gn_gain, gn_bias):
    f32 = jnp.float32
    z = z.astype(f32)
    bsz, seq, _ = z.shape
    r, k, v, wd, ad, gd = jnp.split(
        z, [D_RWKV, 2 * D_RWKV, 3 * D_RWKV, 3 * D_RWKV + DECAY_RANK,
            3 * D_RWKV + DECAY_RANK + ICLR_RANK], axis=-1)
    w_raw = w0 + jnp.tanh(wd) @ w_decay_up
    decay = jnp.exp(-jnp.exp(-jax.nn.softplus(-w_raw) - 0.5))
    iclr = jax.nn.sigmoid(a0 + ad @ w_iclr_up)
    gate = jax.nn.sigmoid(gd) @ w_gate_up
    if vmix is None:
        v_first = v
    else:
        vd, v0, w_vmix_up = vmix
        v = v + (v_first - v) * jax.nn.sigmoid(v0 + vd.astype(f32) @ w_vmix_up)

    def heads(t):
        return t.reshape(bsz, seq, N_HEADS, HEAD_DIM)

    kk = heads(k * k_k)
    kk = kk / jnp.maximum(jnp.sqrt(jnp.sum(kk * kk, axis=-1, keepdims=True)), 1e-12)
    k = heads(k * (1.0 + (iclr - 1.0) * k_a))
    r, v, decay, iclr = heads(r), heads(v), heads(decay), heads(iclr)
    y = wkv7_scan(r, decay, k, v, -kk, kk * iclr)
    mu = jnp.mean(y, axis=-1, keepdims=True)
    var = jnp.mean(jnp.square(y - mu), axis=-1, keepdims=True)
    y = ((y - mu) * lax.rsqrt(var + GN_EPS)).reshape(bsz, seq, D_RWKV) * gn_gain + gn_bias
    bonus = jnp.sum(r * k * r_k, axis=-1, keepdims=True) * v
    y = (y + bonus.reshape(bsz, seq, D_RWKV)) * gate
    return y, v_first


def conformer_conv(z, dw_w, dw_b, ln_gain, ln_bias):
    u_lin, u_gate = jnp.split(z, 2, axis=-1)
    u = u_lin * jax.nn.sigmoid(u_gate)
    c = lax.conv_general_dilated(
        u, dw_w[:, None, :].astype(u.dtype), window_strides=(1,),
        padding=((CONV_WIDTH - 1, 0),), dimension_numbers=('NWC', 'WIO', 'NWC'),
        feature_group_count=D_CONV) + dw_b
    cf = c.astype(jnp.float32)
    mu = jnp.mean(cf, axis=-1, keepdims=True)
    var = jnp.mean(jnp.square(cf - mu), axis=-1, keepdims=True)
    cf = (cf - mu) * lax.rsqrt(var + LN_EPS) * ln_gain + ln_bias
    return jax.nn.silu(cf).astype(z.dtype)


def setup_inputs(seed: int = 0) -> dict:
    key = jax.random.key(seed)
    ks = iter(jax.random.split(key, 40))
    f32 = jnp.float32

    def nrm(shape, scale):
        return scale * jax.random.normal(next(ks), shape, f32)

    def uni(shape):
        return jax.random.uniform(next(ks), shape, f32)

    L, Lv = DEPTH, DEPTH - 1
    return {
        'x': nrm((BATCH, SEQ, D_MODEL), 1.0),
        'p': nrm((DEPTH, BATCH, SEQ, D_PLE), 1.0),
        'norm_mix_pre': 1.0 + nrm((L, D_MODEL), 0.05),
        'norm_mix_post': 1.0 + nrm((L, D_MODEL), 0.05),
        'norm_mlp_pre': 1.0 + nrm((L, D_MODEL), 0.05),
        'norm_mlp_post': 1.0 + nrm((L, D_MODEL), 0.05),
        'w_in': nrm((L, D_MODEL, D_IN), D_MODEL ** -0.5),
        'w_in_vmix': nrm((Lv, D_MODEL, VMIX_RANK), D_MODEL ** -0.5),
        'mu_shift': uni((L, RWKV_COLS)),
        'mu_shift_vmix': uni((Lv, VMIX_RANK)),
        'w0': jnp.linspace(-6.0, -1.0, D_RWKV, dtype=f32)[None, :] + nrm((L, D_RWKV), 0.1),
        'w_decay_up': nrm((L, DECAY_RANK, D_RWKV), DECAY_RANK ** -0.5),
        'a0': nrm((L, D_RWKV), 0.1),
        'w_iclr_up': nrm((L, ICLR_RANK, D_RWKV), ICLR_RANK ** -0.5),
        'v0': nrm((Lv, D_RWKV), 0.1),
        'w_vmix_up': nrm((Lv, VMIX_RANK, D_RWKV), VMIX_RANK ** -0.5),
        'w_gate_up': nrm((L, GATE_RANK, D_RWKV), GATE_RANK ** -0.5),
        'k_k': 0.85 + nrm((L, D_RWKV), 0.05),
        'k_a': 1.0 + nrm((L, D_RWKV), 0.05),
        'r_k': -0.04 + nrm((L, N_HEADS, HEAD_DIM), 0.02),
        'gn_gain': 1.0 + nrm((L, D_RWKV), 0.05),
        'gn_bias': nrm((L, D_RWKV), 0.02),
        'dw_w': nrm((L, CONV_WIDTH, D_CONV), CONV_WIDTH ** -0.5),
        'dw_b': nrm((L, D_CONV), 0.02),
        'conv_ln_gain': 1.0 + nrm((L, D_CONV), 0.05),
        'conv_ln_bias': nrm((L, D_CONV), 0.02),
        'w_out': nrm((L, D_MIX, D_MODEL), D_MIX ** -0.5),
        'w_up': nrm((L, D_MODEL, D_FF), D_MODEL ** -0.5),
        'w_down': nrm((L, D_FF, D_MODEL), D_FF ** -0.5),
        'w_ple': nrm((L, D_PLE, D_MODEL), D_PLE ** -0.5),
        'norm_ple': 1.0 + nrm((L, D_MODEL), 0.05),
        'w_ple_gate': nrm((L, D_MODEL, D_MODEL), D_MODEL ** -0.5),
    }


def reference(x, p, norm_mix_pre, norm_mix_post, norm_mlp_pre, norm_mlp_post,
              w_in, w_in_vmix, mu_shift, mu_shift_vmix, w0, w_decay_up, a0,
              w_iclr_up, v0, w_vmix_up, w_gate_up, k_k, k_a, r_k, gn_gain, gn_bias,
              dw_w, dw_b, conv_ln_gain, conv_ln_bias, w_out, w_up, w_down,
              w_ple, norm_ple, w_ple_gate):
    v_first = None
    for i in range(DEPTH):
        h = rms_norm(x, norm_mix_pre[i])
        if i == 0:
            w_cat = w_in[0]
        else:
            w_cat = jnp.concatenate([w_in[i], w_in_vmix[i - 1]], axis=1)
        z = h @ w_cat
        z_rwkv = lerp_token_shift(z[..., :RWKV_COLS], mu_shift[i])
        z_conv = z[..., RWKV_COLS:D_IN]
        if i == 0:
            vmix = None
        else:
            vmix = (lerp_token_shift(z[..., D_IN:], mu_shift_vmix[i - 1]),
                    v0[i - 1], w_vmix_up[i - 1])
        y_rwkv, v_first = rwkv7_time_mix(
            z_rwkv, vmix, v_first, w0[i], w_decay_up[i], a0[i], w_iclr_up[i],
            w_gate_up[i], k_k[i], k_a[i], r_k[i], gn_gain[i], gn_bias[i])
        y_conv = conformer_conv(z_conv, dw_w[i], dw_b[i], conv_ln_gain[i], conv_ln_bias[i])
        mixed = jnp.concatenate([y_rwkv.astype(x.dtype), y_conv], axis=-1) @ w_out[i]
        x = x + rms_norm(mixed, norm_mix_post[i])
        h = rms_norm(x, norm_mlp_pre[i])
        f = jnp.square(jax.nn.relu(h @ w_up[i])) @ w_down[i]
        x = x + rms_norm(f, norm_mlp_post[i])
        e = rms_norm(p[i] @ w_ple[i], norm_ple[i])
        x = x + jax.nn.sigmoid(x @ w_ple_gate[i]) * e
    return x
```

```python
import numpy as np
from contextlib import ExitStack
import concourse.bass as bass
import concourse.mybir as mybir
from concourse.bass_utils import run_bass_kernel_spmd

F32 = mybir.dt.float32
BF16 = mybir.dt.bfloat16
ALU = mybir.AluOpType
AF = mybir.ActivationFunctionType

D = 2048
DR = 1024
DC = 1024
RC = 3360
DIN = 5408
DZ = 5440
DFF = 8192
DPLE = 256
NPV = 448
NCST = 5 * 128 + 3 * 512 + 128 + 512
SEM_LIMIT = 30000


class Res:
    __slots__ = ("name", "w", "r", "dsem", "const")

    def __init__(self, name):
        self.name = name
        self.w = []
        self.r = {}
        self.dsem = None
        self.const = False


class Tile:
    def __init__(self, t, r):
        self.t = t
        self.r = r

    def __getitem__(self, idx):
        return self.t[idx]


class Sched:
    ENGS = ("sync", "gpsimd", "scalar", "vector", "tensor")

    def __init__(self, nc, stack):
        self.nc = nc
        self.stack = stack
        self.eng = {"sync": nc.sync, "gpsimd": nc.gpsimd, "scalar": nc.scalar,
                    "vector": nc.vector, "tensor": nc.tensor}
        self.nsem = 0
        self.cur = {}
        self.seen = {e: {} for e in self.ENGS}
        self.dsems = []
        self.ninst = 0
        self.rec = None
        self.free = []
        for e in self.ENGS:
            self.cur[e] = self._newsem()

    def _newsem(self):
        s = self.stack.enter_context(self.nc.semaphore("s%d" % self.nsem))
        self.nsem += 1
        return [s, self.nsem, 0]

    def _deps(self, e, reads, writes, append=False):
        need = {}

        def add(ev, same_ok):
            sem, key, val = ev
            if key == self.cur[e][1] and not same_ok:
                return
            if self.seen[e].get(key, 0) >= val:
                return
            if key not in need or need[key][2] < val:
                need[key] = ev

        same_raw = e in ("scalar", "vector", "gpsimd")
        for r in reads:
            for ev in r.w:
                add(ev, same_raw)
        for w in writes:
            if not append:
                for ev in w.w:
                    add(ev, same_raw)
            for ev in w.r.values():
                add(ev, False)
        E = self.eng[e]
        for ev in need.values():
            E.wait_ge(ev[0], ev[2])
            self.seen[e][ev[1]] = ev[2]
            self.ninst += 1

    def _mark(self, ev, reads, writes, append=False):
        for r in reads:
            if not r.const:
                old = r.r.get(ev[1])
                if old is None or old[2] < ev[2]:
                    r.r[ev[1]] = ev
        for w in writes:
            if append:
                w.w = w.w + [ev]
            else:
                w.w = [ev]
                w.r = {}

    def _tick(self, e):
        c = self.cur[e]
        if c[2] >= SEM_LIMIT:
            c = self._newsem()
            self.cur[e] = c
        c[2] += 1
        return (c[0], c[1], c[2])

    def op(self, e, fn, reads=(), writes=(), append=False):
        if self.rec is not None:
            self.rec.append((self.op, (e, fn, reads, writes, append), {}))
            return
        self._deps(e, reads, writes, append)
        ev = self._tick(e)
        fn(self.eng[e]).then_inc(ev[0], 1)
        self.ninst += 1
        self._mark(ev, reads, writes, append)

    def mm(self, fns, reads=(), writes=()):
        if self.rec is not None:
            self.rec.append((self.mm, (fns, reads, writes), {}))
            return
        self._deps("tensor", reads, writes)
        ev = self._tick("tensor")
        n = len(fns)
        for i, fn in enumerate(fns):
            ins = fn(self.nc.tensor)
            if i == n - 1:
                ins.then_inc(ev[0], 1)
        self.ninst += n
        self._mark(ev, reads, writes)

    def dma(self, e, pairs, reads=(), writes=(), **kw):
        if self.rec is not None:
            self.rec.append((self.dma, (e, pairs, reads, writes), kw))
            return
        self._deps(e, reads, writes)
        w0 = writes[0]
        if w0.dsem is None or w0.dsem[2] + 16 * len(pairs) > SEM_LIMIT:
            old = w0.dsem
            if old is None and self.free and self.free[-1][2] + 16 * len(pairs) <= SEM_LIMIT:
                w0.dsem = self.free.pop()
            else:
                w0.dsem = self._newsem()
                self.dsems.append(w0.dsem)
            keep = [(old[0], old[1], old[2])] if old is not None and old[2] > 0 else []
        else:
            keep = []
        ds = w0.dsem
        for (o, i) in pairs:
            self.eng[e].dma_start(out=o, in_=i, **kw).then_inc(ds[0], 16)
            ds[2] += 16
            self.ninst += 1
        ev = (ds[0], ds[1], ds[2])
        for r in reads:
            if not r.const:
                r.r[ev[1]] = ev
        for w in writes:
            w.w = keep + [ev]
            w.r = {}

    def replay_merge(self, A, B):
        assert self.rec is None
        na, nb = len(A), len(B)
        ia = ib = 0
        while ia < na or ib < nb:
            if ib >= nb or (ia < na and ia * nb <= ib * na):
                f, a, k = A[ia]
                ia += 1
            else:
                f, a, k = B[ib]
                ib += 1
            f(*a, **k)

    def release(self, res_list):
        for r in res_list:
            if r.dsem is not None:
                self.free.append(r.dsem)
                r.dsem = None

    def barrier(self):
        evs = []
        for e in self.ENGS:
            c = self.cur[e]
            if c[2] > 0:
                evs.append((c[0], c[1], c[2]))
        for d in self.dsems:
            if d[2] > 0:
                evs.append((d[0], d[1], d[2]))
        for e in self.ENGS:
            for ev in evs:
                if self.seen[e].get(ev[1], 0) < ev[2]:
                    self.eng[e].wait_ge(ev[0], ev[2])
                    self.seen[e][ev[1]] = ev[2]
                    self.ninst += 1


def build(DEPTH, NSEQ, T):
    TOK = NSEQ * T
    NSEG = T // 512
    NT = TOK // 512
    LV = max(DEPTH - 1, 1)
    nc = bass.Bass("TRN2", target_bir_lowering=False)

    def din(name, shape, dt=F32):
        return nc.dram_tensor(name, list(shape), dt, kind="ExternalInput").ap()

    def dscr(name, shape, dt):
        return nc.dram_tensor(name, list(shape), dt, kind="Internal").ap()

    xT_in = din("xT", [D, TOK])
    pT_in = din("pT", [DEPTH, DPLE, TOK])
    w_in = din("w_in", [DEPTH, D, DIN])
    w_vm = din("w_in_vmix", [LV, D, 32])
    w_out = din("w_out", [DEPTH, D, D])
    w_up = din("w_up", [DEPTH, D, DFF])
    w_dn = din("w_down", [DEPTH, DFF, D])
    w_ple = din("w_ple", [DEPTH, DPLE, D])
    w_pg = din("w_ple_gate", [DEPTH, D, D])
    w_du = din("w_decay_up", [DEPTH, 64, DR])
    w_iu = din("w_iclr_up", [DEPTH, 64, DR])
    w_vu = din("w_vmix_up", [LV, 32, DR])
    w_gu = din("w_gate_up", [DEPTH, 160, DR])
    pv_in = din("pv", [DEPTH, 128, NPV])
    cst_in = din("cst", [128, NCST])
    lvl_in = din("lvl", [128, 14 * 512])
    outT = nc.dram_tensor("outT", [D, TOK], F32, kind="ExternalOutput").ap()

    xT = dscr("xTs", [D, TOK], F32)
    zT = dscr("zTs", [DZ, NSEQ, T + 1], F32)
    yT = dscr("yTs", [D, TOK], BF16)
    vfT = dscr("vfTs", [DR, TOK], F32)
    Wb_in = dscr("Wb_in", [DEPTH, D, DZ], BF16)
    Wb_out = dscr("Wb_out", [DEPTH, D, D], BF16)
    Wb_up = dscr("Wb_up", [DEPTH, D, DFF], BF16)
    Wb_dn = dscr("Wb_dn", [DEPTH, DFF, D], BF16)
    Wb_ple = dscr("Wb_ple", [DEPTH, DPLE, D], BF16)
    Wb_pg = dscr("Wb_pg", [DEPTH, D, D], BF16)

    with ExitStack() as top:
        S = Sched(nc, top)
        R_xT, R_zT, R_yT, R_vf, R_W, R_out = (Res(n) for n in ("xT", "zT", "yT", "vf", "W", "out"))
        R_in = Res("inputs")
        R_in.const = True
        RW = {}

        uid = [0]
        scope_res = {}

        def sb(stack, name, shape, dt):
            uid[0] += 1
            name = "%s_u%d" % (name, uid[0])
            t = stack.enter_context(nc.sbuf_tensor(name, list(shape), dt))
            r = Res(name)
            scope_res.setdefault(id(stack), []).append(r)
            return Tile(t, r)

        def ps(stack, name, shape, dt=F32):
            uid[0] += 1
            name = "%s_u%d" % (name, uid[0])
            t = stack.enter_context(nc.psum_tensor(name, list(shape), dt))
            return Tile(t, Res(name))

        V = lambda fn, reads, writes, append=False: S.op("vector", fn, [x.r for x in reads], [x.r for x in writes], append)
        A = lambda fn, reads, writes: S.op("scalar", fn, [x.r for x in reads], [x.r for x in writes])
        G_ = lambda fn, reads, writes: S.op("gpsimd", fn, [x.r for x in reads], [x.r for x in writes])
        MM = lambda fns, reads, writes: S.mm(fns, [x.r for x in reads], [x.r for x in writes])

        cst = sb(top, "cst", [128, NCST], F32)
        pv = sb(top, "pv", [128, DEPTH, NPV], F32)
        matb = sb(top, "matb", [128, 5 * 128], BF16)
        S.dma("sync", [(cst[:], cst_in[:, :])], [R_in], [cst.r])
        lvlm = sb(top, "lvlm", [128, 14 * 512], BF16)
        S.dma("gpsimd", [(lvlm[:], lvl_in[:, :])], [R_in], [lvlm.r])
        lvlm.r.const = True
        S.dma("sync", [(pv[:, l, :], pv_in[l, :, :]) for l in range(DEPTH)], [R_in], [pv.r])
        V(lambda e: e.tensor_copy(out=matb[:], in_=cst[:, 0:640]), [cst], [matb])
        cst.r.const = True
        pv.r.const = True
        matb.r.const = True
        ident = matb[:, 0:128]
        onesD = matb[:, 128:256]
        blk1 = matb[:, 256:384]
        onesC = matb[:, 384:512]
        blkm = matb[:, 512:640]
        mS_st = cst[:, 640:1152]
        mI_st = cst[:, 1152:1664]
        mS_ts = cst[:, 1664:2176]
        ones_f = cst[:, 2176:2304]
        identx4 = cst[:, 2304:2816]

        def pvc(l, c, n=1, p=128):
            return pv[0:p, l, c:c + n]

        with ExitStack() as st0:
            zz = sb(st0, "zz", [128, 8], F32)
            V(lambda e: e.memset(zz[:], 0.0), [], [zz])
            prs = []
            for r0 in range(0, DZ, 128):
                n = min(128, DZ - r0)
                for b in range(NSEQ):
                    prs.append((zT[r0:r0 + n, b, 0:1], zz[0:n, 0:1]))
            S.dma("sync", prs, [zz.r], [R_zT], allow_slow_non_contiguous=True)

            casts = []

            def add_casts(grp, l, kinds):
                for kind in kinds:
                    key = (kind, l)
                    RW[key] = Res("W_%s_%d" % key)
                    if kind == "in":
                        for k0 in range(0, D, 128):
                            ks = slice(k0, k0 + 128)
                            casts.append((grp, key, Wb_in[l, ks, 0:DIN], w_in[l, ks, :]))
                            if l > 0:
                                casts.append((grp, key, Wb_in[l, ks, DIN:DZ], w_vm[l - 1, ks, :]))
                    elif kind == "out":
                        for k0 in range(0, D, 128):
                            casts.append((grp, key, Wb_out[l, k0:k0 + 128, :], w_out[l, k0:k0 + 128, :]))
                    elif kind == "pg":
                        for k0 in range(0, D, 128):
                            casts.append((grp, key, Wb_pg[l, k0:k0 + 128, :], w_pg[l, k0:k0 + 128, :]))
                    elif kind == "up":
                        for k0 in range(0, D, 128):
                            for c0 in range(0, DFF, 2048):
                                casts.append((grp, key, Wb_up[l, k0:k0 + 128, c0:c0 + 2048], w_up[l, k0:k0 + 128, c0:c0 + 2048]))
                    elif kind == "dn":
                        for k0 in range(0, DFF, 256):
                            casts.append((grp, key, Wb_dn[l, k0:k0 + 256, :], w_dn[l, k0:k0 + 256, :]))
                    elif kind == "ple":
                        casts.append((grp, key, Wb_ple[l, :, :], w_ple[l, :, :]))

            add_casts(0, 0, ["in"])
            for LL in range(1, DEPTH + 1):
                add_casts(LL, LL - 1, ["out", "up", "dn", "ple", "pg"])
                if LL < DEPTH:
                    add_casts(LL, LL, ["in"])
            cpos = [0]

            def emit_casts(n=None, grp=None):
                while cpos[0] < len(casts):
                    g, key, o, i = casts[cpos[0]]
                    if grp is not None:
                        if g > grp:
                            break
                    elif n is not None:
                        if n <= 0:
                            break
                        n -= 1
                    S.dma("gpsimd", [(o, i)], [R_in], [RW[key]])
                    cpos[0] += 1

            emit_casts(grp=0)
            S.barrier()

        def tok_stage(L):
            fin = L > 0
            start = L < DEPTH
            l = L - 1
            emit_casts(grp=L)
            with ExitStack() as st:
                xt = sb(st, "xt", [128, 16, 512], F32)
                hT = sb(st, "hT", [128, 16, 512], BF16)
                uT = sb(st, "uT", [128, 32, 512], BF16)
                stg = sb(st, "stg", [128, 16, 512], F32)
                slabs = [sb(st, "slab%d" % i, [128, 16, 512], BF16) for i in range(3)]
                rstd = sb(st, "rstd", [128, 512], F32)
                tmpf = [sb(st, "tmpf%d" % i, [128, 512], F32) for i in range(3)]
                pTb = sb(st, "pTb", [128, 2, 512], BF16)
                acc = [ps(st, "acc%d" % i, [128, 512]) for i in range(6)]
                ssum = ps(st, "ssum", [128, 512])
                cnt = {"slab": 0, "acc": 0, "tmp": 0}

                plan = []
                for _ti in range(NT):
                    if fin:
                        for c0 in range(0, D, 512):
                            plan.append((("out", l), Wb_out[l], 0, c0, 512, 16))
                        for hh in range(2):
                            for c0 in range(0, 4096, 512):
                                plan.append((("up", l), Wb_up[l], 0, hh * 4096 + c0, 512, 16))
                            for c0 in range(0, D, 512):
                                for kq in range(2):
                                    plan.append((("dn", l), Wb_dn[l], hh * 4096 + kq * 2048, c0, 512, 16))
                        for c0 in range(0, D, 512):
                            plan.append((("ple", l), Wb_ple[l], 0, c0, 512, 2))
                        for c0 in range(0, D, 512):
                            plan.append((("pg", l), Wb_pg[l], 0, c0, 512, 16))
                    if start:
                        MZ_ = DZ if L > 0 else DIN
                        for c0 in range(0, MZ_, 512):
                            plan.append((("in", L), Wb_in[L], 0, c0, min(512, MZ_ - c0), 16))
                ppos = {"use": 0, "ld": 0}

                def issue_upto(n):
                    while ppos["ld"] < min(n, len(plan)):
                        key, W, k0, c0, w, kc = plan[ppos["ld"]]
                        sl = slabs[ppos["ld"] % 3]
                        src = W[k0:k0 + kc * 128, c0:c0 + w].rearrange("(c p) m -> p c m", p=128)
                        S.dma("sync", [(sl[:, 0:kc, 0:w], src)], [RW[key]], [sl.r])
                        ppos["ld"] += 1

                def load_slab(key, k0, c0, w, kc=16):
                    i = ppos["use"]
                    pk, W, pk0, pc0, pw, pkc = plan[i]
                    assert (pk, pk0, pc0, pw, pkc) == (key, k0, c0, w, kc), (plan[i][0], plan[i][2:], key, k0, c0, w, kc)
                    issue_upto(i + 3)
                    ppos["use"] += 1
                    return slabs[i % 3]

                def next_acc():
                    a = acc[cnt["acc"] % 6]
                    cnt["acc"] += 1
                    return a

                def next_tmp():
                    a = tmpf[cnt["tmp"] % 3]
                    cnt["tmp"] += 1
                    return a

                def norm_stats(src, scr=None, off=0):
                    scr = hT if scr is None else scr
                    A(lambda e: e.activation(out=scr[:, off:off + 9, :], in_=src[:, 0:9, :], func=AF.Square), [src], [scr])
                    V(lambda e: e.tensor_tensor(out=scr[:, off + 9:off + 16, :], in0=src[:, 9:16, :], in1=src[:, 9:16, :], op=ALU.mult),
                      [src], [scr], append=True)
                    MM([(lambda e, c=c: e.matmul(ssum[:], lhsT=onesD, rhs=scr[:, off + c, :],
                                                 start=(c == 0), stop=(c == 15))) for c in range(16)],
                       [scr, matb], [ssum])
                    A(lambda e: e.activation(out=rstd[:], in_=ssum[:], func=AF.Ln, bias=1e-6, scale=1.0),
                      [ssum], [rstd])
                    A(lambda e: e.activation(out=rstd[:], in_=rstd[:], func=AF.Exp, scale=-0.5), [rstd], [rstd])

                def scale_by(dst, src, gcol, ll):
                    for c in range(16):
                        V(lambda e, c=c: e.scalar_tensor_tensor(
                            out=dst[:, c, :], in0=src[:, c, :], scalar=pvc(ll, gcol + c), in1=rstd[:],
                            op0=ALU.mult, op1=ALU.mult), [src, rstd, pv], [dst])

                def add_into_x(src):
                    V(lambda e: e.tensor_tensor(out=xt[:], in0=xt[:], in1=src[:], op=ALU.add), [xt, src], [xt])

                def dense(key, rhsT, KC, M, evac, cbase=0):
                    for c0 in range(0, M, 512):
                        w = min(512, M - c0)
                        sl = load_slab(key, 0, cbase + c0, w, kc=KC)
                        for m0 in range(0, w, 128):
                            msz = min(128, w - m0)
                            a = next_acc()
                            MM([(lambda e, c=c, a=a, m0=m0, msz=msz, sl=sl: e.matmul(
                                a[0:msz, :], lhsT=sl[:, c, m0:m0 + msz], rhs=rhsT[:, c, :],
                                start=(c == 0), stop=(c == KC - 1))) for c in range(KC)],
                               [sl, rhsT], [a])
                            evac((c0 + m0) // 128, msz, a)

                def load_y(tj):
                    ysrc = yT[:, tj * 512:tj * 512 + 512].rearrange("(c p) t -> p c t", p=128)
                    S.dma("sync", [(uT[:, 0:16, :], ysrc)], [R_yT], [uT.r])

                for ti in range(NT):
                    b = ti // NSEG
                    seg = ti % NSEG
                    t0 = ti * 512
                    xsrc = (xT_in if L == 0 else xT)[:, t0:t0 + 512].rearrange("(c p) t -> p c t", p=128)
                    S.dma("sync", [(xt[:], xsrc)], [R_in if L == 0 else R_xT], [xt.r])
                    if fin:
                        if ti == 0:
                            load_y(0)
                        for c in range(2):
                            tm = next_tmp()
                            S.dma("sync", [(tm[:], pT_in[l, c * 128:(c + 1) * 128, t0:t0 + 512])], [R_in], [tm.r])
                            V(lambda e, c=c, tm=tm: e.tensor_copy(out=pTb[:, c, :], in_=tm[:]), [tm], [pTb])

                        def ev_copy(mi, msz, a):
                            A(lambda e: e.activation(out=stg[0:msz, mi, :], in_=a[0:msz, :], func=AF.Copy),
                              [a], [stg])
                        dense(("out", l), uT, 16, D, ev_copy)
                        norm_stats(stg)
                        scale_by(stg, stg, 16, l)
                        add_into_x(stg)
                        norm_stats(xt)
                        scale_by(hT, xt, 32, l)
                        for hh in range(2):
                            def ev_relu2(mi, msz, a):
                                tm = next_tmp()
                                A(lambda e: e.activation(out=tm[:], in_=a[:], func=AF.Relu), [a], [tm])
                                V(lambda e: e.tensor_tensor(out=uT[:, mi, :], in0=tm[:], in1=tm[:], op=ALU.mult),
                                  [tm], [uT])
                            dense(("up", l), hT, 16, 4096, ev_relu2, cbase=hh * 4096)
                            for c0 in range(0, D, 512):
                                accs = [next_acc() for _ in range(4)]
                                for kq in range(2):
                                    sl = load_slab(("dn", l), hh * 4096 + kq * 2048, c0, 512)
                                    for mi in range(4):
                                        a = accs[mi]
                                        MM([(lambda e, c=c, a=a, mi=mi, sl=sl, kq=kq: e.matmul(
                                            a[:], lhsT=sl[:, c, mi * 128:(mi + 1) * 128], rhs=uT[:, kq * 16 + c, :],
                                            start=(kq == 0 and c == 0), stop=(kq == 1 and c == 15)))
                                            for c in range(16)], [sl, uT], [a])
                                for mi in range(4):
                                    a = accs[mi]
                                    m = c0 // 128 + mi
                                    if hh == 0:
                                        A(lambda e, a=a, m=m: e.activation(out=stg[:, m, :], in_=a[:], func=AF.Copy),
                                          [a], [stg])
                                    else:
                                        V(lambda e, a=a, m=m: e.tensor_tensor(out=stg[:, m, :], in0=stg[:, m, :],
                                                                              in1=a[:], op=ALU.add), [a, stg], [stg])
                        if ti + 1 < NT:
                            load_y(ti + 1)
                        norm_stats(stg)
                        scale_by(stg, stg, 48, l)
                        add_into_x(stg)
                        A(lambda e: e.activation(out=hT[:], in_=xt[:], func=AF.Copy), [xt], [hT])
                        dense(("ple", l), pTb, 2, D, ev_copy)
                        norm_stats(stg, uT, 16)
                        scale_by(stg, stg, 64, l)

                        def ev_gate(mi, msz, a):
                            tm = next_tmp()
                            A(lambda e: e.activation(out=tm[:], in_=a[:], func=AF.Sigmoid), [a], [tm])
                            V(lambda e: e.tensor_tensor(out=stg[:, mi, :], in0=stg[:, mi, :], in1=tm[:], op=ALU.mult),
                              [tm, stg], [stg])
                        dense(("pg", l), hT, 16, D, ev_gate)
                        add_into_x(stg)
                    if start:
                        norm_stats(xt)
                        scale_by(hT, xt, 0, L)
                        MZ = DZ if L > 0 else DIN

                        def ev_z(mi, msz, a):
                            tm = next_tmp()
                            A(lambda e: e.activation(out=tm[0:msz, :], in_=a[0:msz, :], func=AF.Copy), [a], [tm])
                            S.dma("sync", [(zT[mi * 128:mi * 128 + msz, b, 1 + seg * 512:1 + seg * 512 + 512],
                                              tm[0:msz, :])], [tm.r], [R_zT])
                        dense(("in", L), hT, 16, MZ, ev_z)
                    dst = (outT if L == DEPTH else xT)[:, t0:t0 + 512].rearrange("(c p) t -> p c t", p=128)
                    S.dma("sync", [(dst, xt[:])], [xt.r], [R_out if L == DEPTH else R_xT])
                    emit_casts(n=14)
                S.barrier()
                S.release(scope_res.pop(id(st), []))

        def seq_stage(L):
            with ExitStack() as st:
                wdu = sb(st, "wdu", [64, DR], BF16)
                wiu = sb(st, "wiu", [64, DR], BF16)
                wvu = sb(st, "wvu", [32, DR], BF16)
                wg0 = sb(st, "wg0", [128, DR], BF16)
                wg1 = sb(st, "wg1", [32, DR], BF16)
                lst = [(wdu, w_du[L], 64), (wiu, w_iu[L], 64), (wg0, w_gu[L, 0:128, :], 128),
                       (wg1, w_gu[L, 128:160, :], 32)]
                if L > 0:
                    lst.append((wvu, w_vu[L - 1], 32))
                lwst = ExitStack()
                lwf = sb(lwst, "lwf", [128, DR], F32)
                for (dstt, src, n) in lst:
                    S.dma("sync", [(lwf[0:n, :], src)], [R_in], [lwf.r])
                    V(lambda e, dstt=dstt, n=n: e.tensor_copy(out=dstt[0:n, :], in_=lwf[0:n, :]), [lwf], [dstt])
                S.barrier()
                S.release(scope_res.pop(id(lwst), []))
                lwst.close()
                sm_raw0 = sb(st, "sm_raw0", [128, 513], F32)
                sm_t = sb(st, "sm_t", [128, 512], F32)
                twd = sb(st, "twd", [64, 512], BF16)
                adb = sb(st, "adb", [64, 512], BF16)
                sg0 = sb(st, "sg0", [128, 512], BF16)
                sg1 = sb(st, "sg1", [32, 512], BF16)
                vdb = sb(st, "vdb", [32, 512], BF16)
                rawS = [[sb(st, "raw%d_%d" % (k, i), [128, 513], F32) for i in range(3)] for k in range(2)]
                f = {n: sb(st, "f_" + n, [128, 512], F32) for n in
                     ("r", "k", "v", "t1", "t2", "t3", "g", "eg", "kkn", "icl", "k2", "bb")}
                sqb = sb(st, "sqb", [128, 512], BF16)
                NP = 4
                RT = [sb(st, "RT%d" % i, [128, 512], BF16) for i in range(NP)]
                AT = [sb(st, "AT%d" % i, [128, 512], BF16) for i in range(NP)]
                BT = [sb(st, "BT%d" % i, [128, 512], BF16) for i in range(NP)]
                KT = [sb(st, "KT%d" % i, [128, 512], BF16) for i in range(NP)]
                KH = sb(st, "KH", [128, 512], BF16)
                BH = sb(st, "BH", [128, 512], BF16)
                VB = sb(st, "VB", [128, 512], BF16)
                Ktok = [sb(st, "Ktok%d" % i, [128, 4, 128], BF16) for i in range(NP)]
                Btok = [sb(st, "Btok%d" % i, [128, 4, 128], BF16) for i in range(NP)]
                Vtok = [sb(st, "Vtok%d" % i, [128, 4, 128], BF16) for i in range(NP)]
                bonus = [sb(st, "bonus%d" % i, [128, 512], F32) for i in range(NP)]
                egC = [sb(st, "egC%d" % i, [128, 4], F32) for i in range(NP)]
                Yt = [sb(st, "Yt%d" % i, [128, 512], F32) for i in range(NP)]
                AakT = [sb(st, "AakT%d" % i, [128, 512], BF16) for i in range(2 * NP)]
                ArbT = [sb(st, "ArbT%d" % i, [128, 512], BF16) for i in range(2 * NP)]
                ArkT = [sb(st, "ArkT%d" % i, [128, 512], BF16) for i in range(2 * NP)]
                TT = [sb(st, "TT%d" % i, [128, 512], BF16) for i in range(2 * NP)]
                Mh = [sb(st, "Mh%d" % i, [128, 512], BF16) for i in range(4)]
                Ph = [sb(st, "Ph%d" % i, [128, 512], BF16) for i in range(4)]
                Twh = [[sb(st, "Tw%d_%d" % (i, j), [128, 512], BF16) for j in range(2)] for i in range(4)]
                TTwh = [[sb(st, "TTw%d_%d" % (i, j), [128, 512], BF16) for j in range(2)] for i in range(4)]
                Zbh = [[sb(st, "Zb%d_%d" % (i, j), [128, 512], BF16) for j in range(2)] for i in range(4)]
                H32 = sb(st, "H32", [128, NP, 64], F32)
                Hd = sb(st, "Hd", [128, NP, 64], F32)
                HbE = sb(st, "HbE", [128, NP, 64], BF16)
                HbO = sb(st, "HbO", [128, NP, 64], BF16)
                Wsb = sb(st, "Wsb", [128, 2 * NP, 64], BF16)
                Usb = sb(st, "Usb", [128, 2 * NP, 64], BF16)
                pp = [ps(st, "pp%d" % i, [128, 512]) for i in range(5)]
                pq = [ps(st, "pq%d" % i, [128, 512]) for i in range(2)]
                ptr = ps(st, "ptr", [128, 8, 128], BF16)
                pc = {"p": 0, "q": 0}

                def npp():
                    a = pp[pc["p"] % 5]
                    pc["p"] += 1
                    return a

                def nq():
                    a = pq[pc["q"] % 2]
                    pc["q"] += 1
                    return a

                def rsqrt_to(dst, src_ps, scale, bias):
                    A(lambda e: e.activation(out=dst[:], in_=src_ps[:], func=AF.Ln, bias=bias, scale=scale),
                      [src_ps], [dst])
                    A(lambda e: e.activation(out=dst[:], in_=dst[:], func=AF.Exp, scale=-0.5), [dst], [dst])

                for b in range(NSEQ):
                    for bt in range(8 // NP):
                        V(lambda e: e.memset(H32[:], 0.0), [], [H32])
                        V(lambda e: e.memset(HbE[:], 0.0), [], [HbE])
                        V(lambda e: e.memset(HbO[:], 0.0), [], [HbO])
                        for seg in range(NSEG):
                            emit_casts(n=30)
                            c0 = seg * 512
                            t0 = b * T + seg * 512
                            smalls = [(3072, 64, 104, AF.Tanh, twd), (3136, 64, 105, AF.Copy, adb),
                                      (3200, 128, 106, AF.Sigmoid, sg0), (3328, 32, 107, AF.Sigmoid, sg1)]
                            if L > 0:
                                smalls.append((5408, 32, 108, AF.Copy, vdb))
                            for si_, (r0, n, mc, fn, dstt) in enumerate(smalls):
                                sm_raw = (sm_raw0, rawS[1][0], rawS[1][1])[si_ % 3]
                                S.dma("sync", [(sm_raw[0:n, :], zT[r0:r0 + n, b, c0:c0 + 513])], [R_zT], [sm_raw.r])
                                V(lambda e, n=n: e.tensor_tensor(out=sm_t[0:n, :], in0=sm_raw[0:n, 0:512],
                                                                 in1=sm_raw[0:n, 1:513], op=ALU.subtract),
                                  [sm_raw], [sm_t])
                                V(lambda e, n=n, mc=mc: e.scalar_tensor_tensor(
                                    out=sm_t[0:n, :], in0=sm_t[0:n, :], scalar=pvc(L, mc, 1, n), in1=sm_raw[0:n, 1:513],
                                    op0=ALU.mult, op1=ALU.add), [sm_t, sm_raw, pv], [sm_t])
                                A(lambda e, n=n, fn=fn, dstt=dstt: e.activation(out=dstt[0:n, :], in_=sm_t[0:n, :], func=fn),
                                  [sm_t], [dstt])
                            def load_raw(pj):
                                roj = (bt * NP + pj) * 128
                                for j in range(3):
                                    S.dma("sync", [(rawS[pj % 2][j][:], zT[j * DR + roj:j * DR + roj + 128, b, c0:c0 + 513])],
                                          [R_zT], [rawS[pj % 2][j].r])

                            def prep_pair(pi):
                                PO = G_ if L > 0 else V
                                pr = bt * NP + pi
                                ro = pr * 128
                                raw = rawS[pi % 2]
                                if pi == 0:
                                    load_raw(0)
                                if pi + 1 < NP:
                                    load_raw(pi + 1)
                                for j, nm in enumerate(("r", "k", "v")):
                                    V(lambda e, j=j: e.tensor_tensor(out=f["t1"][:], in0=raw[j][:, 0:512],
                                                                     in1=raw[j][:, 1:513], op=ALU.subtract),
                                      [raw[j]], [f["t1"]])
                                    V(lambda e, j=j, nm=nm: e.scalar_tensor_tensor(
                                        out=f[nm][:], in0=f["t1"][:], scalar=pvc(L, 80 + 8 * j + pr), in1=raw[j][:, 1:513],
                                        op0=ALU.mult, op1=ALU.add), [f["t1"], raw[j], pv], [f[nm]])
                                a = nq()
                                MM([lambda e, a=a: e.matmul(a[:], lhsT=wdu[0:64, ro:ro + 128], rhs=twd[0:64, :],
                                                            start=True, stop=True)], [wdu, twd], [a])
                                A(lambda e, a=a: e.activation(out=f["t1"][:], in_=a[:], func=AF.Sigmoid,
                                                              bias=pvc(L, 112 + pr), scale=1.0), [a, pv], [f["t1"]])
                                V(lambda e: e.tensor_scalar(out=f["t1"][:], in0=f["t1"][:], scalar1=-0.6065306597126334,
                                                            scalar2=None, op0=ALU.mult), [f["t1"]], [f["t1"]])
                                for c in range(4):
                                    V(lambda e, c=c: e.tensor_tensor_scan(
                                        out=f["g"][:, c * 128:(c + 1) * 128], data0=ones_f,
                                        data1=f["t1"][:, c * 128:(c + 1) * 128], initial=0.0,
                                        op0=ALU.mult, op1=ALU.add), [f["t1"], cst], [f["g"]])
                                A(lambda e: e.activation(out=f["eg"][:], in_=f["g"][:], func=AF.Exp), [f["g"]], [f["eg"]])
                                V(lambda e, pi=pi: e.tensor_copy(
                                    out=egC[pi][:], in_=f["eg"][:].rearrange("p (c t) -> p c t", t=128)[:, :, 127]),
                                  [f["eg"]], [egC[pi]])
                                V(lambda e, pi=pi: e.tensor_tensor(out=RT[pi][:], in0=f["r"][:], in1=f["eg"][:], op=ALU.mult),
                                  [f["r"], f["eg"]], [RT[pi]])
                                V(lambda e: e.tensor_tensor(out=f["t2"][:], in0=f["g"][:], in1=f["t1"][:], op=ALU.subtract),
                                  [f["g"], f["t1"]], [f["t2"]])
                                A(lambda e: e.activation(out=f["t2"][:], in_=f["t2"][:], func=AF.Exp), [f["t2"]], [f["t2"]])
                                A(lambda e: e.activation(out=f["t3"][:], in_=f["g"][:], func=AF.Exp, scale=-1.0),
                                  [f["g"]], [f["t3"]])
                                for c in range(4):
                                    V(lambda e, c=c: e.tensor_scalar(
                                        out=f["eg"][:, c * 128:(c + 1) * 128], in0=f["g"][:, c * 128:(c + 1) * 128],
                                        scalar1=f["g"][:, c * 128 + 127:c * 128 + 128], scalar2=-1.0,
                                        op0=ALU.subtract, op1=ALU.mult), [f["g"]], [f["eg"]])
                                A(lambda e: e.activation(out=f["eg"][:], in_=f["eg"][:], func=AF.Exp), [f["eg"]], [f["eg"]])
                                a = nq()
                                MM([lambda e, a=a: e.matmul(a[:], lhsT=wiu[0:64, ro:ro + 128], rhs=adb[0:64, :],
                                                            start=True, stop=True)], [wiu, adb], [a])
                                A(lambda e, a=a: e.activation(out=f["icl"][:], in_=a[:], func=AF.Sigmoid,
                                                              bias=pvc(L, 120 + pr), scale=1.0), [a, pv], [f["icl"]])
                                if L > 0:
                                    a = nq()
                                    MM([lambda e, a=a: e.matmul(a[:], lhsT=wvu[0:32, ro:ro + 128], rhs=vdb[0:32, :],
                                                                start=True, stop=True)], [wvu, vdb], [a])
                                    A(lambda e, a=a: e.activation(out=f["t1"][:], in_=a[:], func=AF.Sigmoid,
                                                                  bias=pvc(L, 128 + pr), scale=1.0), [a, pv], [f["t1"]])
                                    vfb = Yt[pi]
                                    S.dma("sync", [(vfb[:], vfT[ro:ro + 128, t0:t0 + 512])], [R_vf], [vfb.r])
                                    V(lambda e: e.tensor_tensor(out=vfb[:], in0=vfb[:], in1=f["v"][:], op=ALU.subtract),
                                      [vfb, f["v"]], [vfb])
                                    V(lambda e: e.tensor_tensor(out=vfb[:], in0=vfb[:], in1=f["t1"][:], op=ALU.mult),
                                      [vfb, f["t1"]], [vfb])
                                    V(lambda e: e.tensor_tensor(out=f["v"][:], in0=f["v"][:], in1=vfb[:], op=ALU.add),
                                      [f["v"], vfb], [f["v"]])
                                else:
                                    S.dma("sync", [(vfT[ro:ro + 128, t0:t0 + 512], f["v"][:])], [f["v"].r], [R_vf])
                                V(lambda e: e.tensor_copy(out=VB[:], in_=f["v"][:]), [f["v"]], [VB])
                                V(lambda e: e.tensor_scalar(out=f["kkn"][:], in0=f["k"][:], scalar1=pvc(L, 136 + pr),
                                                            scalar2=None, op0=ALU.mult), [f["k"], pv], [f["kkn"]])
                                A(lambda e: e.activation(out=sqb[:], in_=f["kkn"][:], func=AF.Square), [f["kkn"]], [sqb])
                                a = nq()
                                MM([lambda e, a=a: e.matmul(a[:], lhsT=blk1, rhs=sqb[:], start=True, stop=True)],
                                   [matb, sqb], [a])
                                rsqrt_to(f["t1"], a, 1.0, 1e-12)
                                V(lambda e: e.tensor_tensor(out=f["kkn"][:], in0=f["kkn"][:], in1=f["t1"][:], op=ALU.mult),
                                  [f["kkn"], f["t1"]], [f["kkn"]])
                                V(lambda e: e.tensor_scalar(out=f["t1"][:], in0=f["icl"][:], scalar1=-1.0,
                                                            scalar2=pvc(L, 144 + pr), op0=ALU.add, op1=ALU.mult),
                                  [f["icl"], pv], [f["t1"]])
                                V(lambda e: e.scalar_tensor_tensor(out=f["k2"][:], in0=f["t1"][:], scalar=1.0, in1=f["k"][:],
                                                                   op0=ALU.add, op1=ALU.mult), [f["t1"], f["k"]], [f["k2"]])
                                V(lambda e: e.tensor_tensor(out=f["bb"][:], in0=f["kkn"][:], in1=f["icl"][:], op=ALU.mult),
                                  [f["kkn"], f["icl"]], [f["bb"]])
                                PO(lambda e, pi=pi: e.tensor_tensor(out=KT[pi][:], in0=f["k2"][:], in1=f["t3"][:], op=ALU.mult),
                                  [f["k2"], f["t3"]], [KT[pi]])
                                PO(lambda e, pi=pi: e.tensor_tensor(out=BT[pi][:], in0=f["bb"][:], in1=f["t3"][:], op=ALU.mult),
                                  [f["bb"], f["t3"]], [BT[pi]])
                                V(lambda e, pi=pi: e.scalar_tensor_tensor(out=AT[pi][:], in0=f["kkn"][:], scalar=-1.0,
                                                                          in1=f["t2"][:], op0=ALU.mult, op1=ALU.mult),
                                  [f["kkn"], f["t2"]], [AT[pi]])
                                PO(lambda e: e.tensor_tensor(out=KH[:], in0=f["k2"][:], in1=f["eg"][:], op=ALU.mult),
                                  [f["k2"], f["eg"]], [KH])
                                PO(lambda e: e.tensor_tensor(out=BH[:], in0=f["bb"][:], in1=f["eg"][:], op=ALU.mult),
                                  [f["bb"], f["eg"]], [BH])
                                V(lambda e: e.scalar_tensor_tensor(out=sqb[:], in0=f["r"][:], scalar=pvc(L, 152 + pr),
                                                                   in1=f["k2"][:], op0=ALU.mult, op1=ALU.mult),
                                  [f["r"], f["k2"], pv], [sqb])
                                a = nq()
                                MM([lambda e, a=a: e.matmul(a[:], lhsT=blk1, rhs=sqb[:], start=True, stop=True)],
                                   [matb, sqb], [a])
                                V(lambda e, a=a, pi=pi: e.tensor_tensor(out=bonus[pi][:], in0=a[:], in1=f["v"][:], op=ALU.mult),
                                  [a, f["v"]], [bonus[pi]])
                                for (srcT, dstT) in ((KH, Ktok[pi]), (BH, Btok[pi]), (VB, Vtok[pi])):
                                    MM([(lambda e, c=c, srcT=srcT: e.transpose(ptr[:, c, :], srcT[:, c * 128:(c + 1) * 128], ident))
                                        for c in range(4)], [srcT, matb], [ptr])
                                    A(lambda e, dstT=dstT: e.activation(out=dstT[:], in_=ptr[:, 0:4, :], func=AF.Copy),
                                      [ptr], [dstT])
                            lvs = lambda i: lvlm[:, i * 512:(i + 1) * 512]
                            G = 4
                            def inv_group(g0):
                                heads = list(range(g0, g0 + G))

                                def prod_k(hd, lT, rT, mask, dst):
                                    par = hd % 2
                                    ks = slice(par * 64, par * 64 + 64)
                                    a = npp()
                                    MM([(lambda e, c=c, a=a: e.matmul(
                                        a[:, c * 128:(c + 1) * 128], lhsT=lT[ks, c * 128:(c + 1) * 128],
                                        rhs=rT[ks, c * 128:(c + 1) * 128], start=True, stop=True)) for c in range(4)],
                                       [lT, rT], [a])
                                    V(lambda e, a=a: e.tensor_tensor(out=dst[:], in0=a[:], in1=mask, op=ALU.mult),
                                      [a, cst], [dst])

                                for hd in heads:
                                    pi = hd // 2
                                    prod_k(hd, AT[pi], BT[pi], mS_ts, Mh[hd - g0])
                                    prod_k(hd, BT[pi], AT[pi], mS_st, Ph[hd - g0])
                                for hd in heads:
                                    pi = hd // 2
                                    prod_k(hd, KT[pi], AT[pi], mS_st, AakT[hd])
                                    prod_k(hd, BT[pi], RT[pi], mI_st, ArbT[hd])
                                    prod_k(hd, KT[pi], RT[pi], mI_st, ArkT[hd])
                                cur = {}
                                TI = V if L == 0 else G_
                                for hd in heads:
                                    i = hd - g0
                                    Tc, TTc = Twh[i][0], TTwh[i][0]
                                    TI(lambda e, Tc=Tc, i=i: e.tensor_tensor(out=Tc[:], in0=Mh[i][:], in1=lvs(0), op=ALU.mult), [Mh[i], lvlm], [Tc])
                                    TI(lambda e, Tc=Tc: e.tensor_tensor(out=Tc[:], in0=Tc[:], in1=identx4, op=ALU.add), [Tc, cst], [Tc])
                                    TI(lambda e, TTc=TTc, i=i: e.tensor_tensor(out=TTc[:], in0=Ph[i][:], in1=lvs(1), op=ALU.mult), [Ph[i], lvlm], [TTc])
                                    TI(lambda e, TTc=TTc: e.tensor_tensor(out=TTc[:], in0=TTc[:], in1=identx4, op=ALU.add), [TTc, cst], [TTc])
                                    cur[hd] = (Tc, TTc)
                                for k in range(1, 7):
                                    last = k == 6
                                    for hd in heads:
                                        i = hd - g0
                                        Tc, TTc = cur[hd]
                                        a = npp()
                                        MM([(lambda e, c=c, a=a, TTc=TTc, i=i: e.matmul(
                                            a[:, c * 128:(c + 1) * 128], lhsT=Mh[i][:, c * 128:(c + 1) * 128],
                                            rhs=TTc[:, c * 128:(c + 1) * 128], start=True, stop=True)) for c in range(4)],
                                           [Mh[i], TTc], [a])
                                        V(lambda e, a=a, k=k, i=i: e.tensor_tensor(out=Zbh[i][1][:], in0=a[:], in1=lvs(2 * k + 1), op=ALU.mult),
                                          [a, lvlm], [Zbh[i][1]])
                                        if not last:
                                            a = npp()
                                            MM([(lambda e, c=c, a=a, Tc=Tc, i=i: e.matmul(
                                                a[:, c * 128:(c + 1) * 128], lhsT=Ph[i][:, c * 128:(c + 1) * 128],
                                                rhs=Tc[:, c * 128:(c + 1) * 128], start=True, stop=True)) for c in range(4)],
                                               [Ph[i], Tc], [a])
                                            V(lambda e, a=a, k=k, i=i: e.tensor_tensor(out=Zbh[i][0][:], in0=a[:], in1=lvs(2 * k), op=ALU.mult),
                                              [a, lvlm], [Zbh[i][0]])
                                    for hd in heads:
                                        i = hd - g0
                                        Tc, TTc = cur[hd]
                                        Tn = Twh[i][k % 2]
                                        TTn = TT[hd] if last else TTwh[i][k % 2]
                                        a3 = npp()
                                        fns = []
                                        for c in range(4):
                                            fns.append(lambda e, c=c, a3=a3, Tc=Tc, i=i: e.matmul(
                                                a3[:, c * 128:(c + 1) * 128], lhsT=Tc[:, c * 128:(c + 1) * 128],
                                                rhs=Zbh[i][1][:, c * 128:(c + 1) * 128], start=True, stop=False))
                                            fns.append(lambda e, c=c, a3=a3, TTc=TTc: e.matmul(
                                                a3[:, c * 128:(c + 1) * 128], lhsT=ident,
                                                rhs=TTc[:, c * 128:(c + 1) * 128], start=False, stop=True))
                                        MM(fns, [Tc, TTc, Zbh[i][1], matb], [a3])
                                        A(lambda e, a3=a3, TTn=TTn: e.activation(out=TTn[:], in_=a3[:], func=AF.Copy), [a3], [TTn])
                                        if not last:
                                            a3 = npp()
                                            fns = []
                                            for c in range(4):
                                                fns.append(lambda e, c=c, a3=a3, TTc=TTc, i=i: e.matmul(
                                                    a3[:, c * 128:(c + 1) * 128], lhsT=TTc[:, c * 128:(c + 1) * 128],
                                                    rhs=Zbh[i][0][:, c * 128:(c + 1) * 128], start=True, stop=False))
                                                fns.append(lambda e, c=c, a3=a3, Tc=Tc: e.matmul(
                                                    a3[:, c * 128:(c + 1) * 128], lhsT=ident,
                                                    rhs=Tc[:, c * 128:(c + 1) * 128], start=False, stop=True))
                                            MM(fns, [Tc, TTc, Zbh[i][0], matb], [a3])
                                            A(lambda e, a3=a3, Tn=Tn: e.activation(out=Tn[:], in_=a3[:], func=AF.Copy), [a3], [Tn])
                                        cur[hd] = (Tn, TTn)
                            prep_pair(0)
                            prep_pair(1)
                            S.rec = []
                            prep_pair(2)
                            prep_pair(3)
                            recA = S.rec
                            S.rec = []
                            inv_group(0)
                            recB = S.rec
                            S.rec = None
                            S.replay_merge(recA, recB)
                            inv_group(G)
                            for c in range(4):
                                cs = slice(c * 128, (c + 1) * 128)
                                yps = npp()
                                wps = npp()
                                fns = []
                                for hd in range(2 * NP):
                                    pi, par = hd // 2, hd % 2
                                    Hb = HbO if par else HbE
                                    fns.append(lambda e, hd=hd, pi=pi, Hb=Hb: e.matmul(
                                        wps[:, hd * 64:(hd + 1) * 64], lhsT=AT[pi][:, cs], rhs=Hb[:, pi, :],
                                        start=True, stop=False))
                                    fns.append(lambda e, hd=hd, pi=pi, par=par: e.matmul(
                                        wps[:, hd * 64:(hd + 1) * 64], lhsT=AakT[hd][:, cs],
                                        rhs=Vtok[pi][:, c, par * 64:par * 64 + 64], start=False, stop=True))
                                MM(fns, AT + [HbE, HbO] + AakT + Vtok, [wps])
                                V(lambda e, wps=wps: e.tensor_copy(out=Wsb[:].rearrange("p h v -> p (h v)"), in_=wps[:]),
                                  [wps], [Wsb])
                                for pi in range(NP):
                                    V(lambda e, pi=pi, c=c: e.tensor_scalar(out=Hd[:, pi, :], in0=H32[:, pi, :],
                                                                            scalar1=egC[pi][:, c:c + 1], scalar2=None, op0=ALU.mult),
                                      [H32, egC[pi]], [Hd], append=(pi > 0))
                                ups = npp()
                                MM([(lambda e, hd=hd: e.matmul(ups[:, hd * 64:(hd + 1) * 64], lhsT=TT[hd][:, cs],
                                                               rhs=Wsb[:, hd, :], start=True, stop=True))
                                    for hd in range(2 * NP)], TT + [Wsb], [ups])
                                A(lambda e, ups=ups: e.activation(out=Usb[:].rearrange("p h v -> p (h v)"), in_=ups[:],
                                                                  func=AF.Copy), [ups], [Usb])
                                fns = []
                                for hd in range(2 * NP):
                                    pi, par = hd // 2, hd % 2
                                    Hb = HbO if par else HbE
                                    o = lambda: yps[par * 64:par * 64 + 64, pi * 128:(pi + 1) * 128]
                                    fns.append(lambda e, pi=pi, par=par, Hb=Hb: e.matmul(
                                        yps[par * 64:par * 64 + 64, pi * 128:(pi + 1) * 128], lhsT=Hb[:, pi, :],
                                        rhs=RT[pi][:, cs], start=True, stop=False))
                                    fns.append(lambda e, hd=hd, pi=pi, par=par: e.matmul(
                                        yps[par * 64:par * 64 + 64, pi * 128:(pi + 1) * 128], lhsT=Usb[:, hd, :],
                                        rhs=ArbT[hd][:, cs], start=False, stop=False))
                                    fns.append(lambda e, hd=hd, pi=pi, par=par: e.matmul(
                                        yps[par * 64:par * 64 + 64, pi * 128:(pi + 1) * 128],
                                        lhsT=Vtok[pi][:, c, par * 64:par * 64 + 64],
                                        rhs=ArkT[hd][:, cs], start=False, stop=True))
                                MM(fns, [HbE, HbO, Usb] + RT + ArbT + ArkT + Vtok, [yps])
                                for pi in range(NP):
                                    A(lambda e, pi=pi: e.activation(out=Yt[pi][:, cs], in_=yps[:, pi * 128:(pi + 1) * 128],
                                                                    func=AF.Copy), [yps], [Yt[pi]])
                                hps = npp()
                                fns = []
                                for hd in range(2 * NP):
                                    pi, par = hd // 2, hd % 2
                                    fns.append(lambda e, hd=hd, pi=pi, par=par: e.matmul(
                                        hps[par * 64:par * 64 + 64, pi * 64:(pi + 1) * 64],
                                        lhsT=Btok[pi][:, c, par * 64:par * 64 + 64], rhs=Usb[:, hd, :],
                                        start=True, stop=False))
                                    fns.append(lambda e, hd=hd, pi=pi, par=par: e.matmul(
                                        hps[par * 64:par * 64 + 64, pi * 64:(pi + 1) * 64],
                                        lhsT=Ktok[pi][:, c, par * 64:par * 64 + 64],
                                        rhs=Vtok[pi][:, c, par * 64:par * 64 + 64], start=False, stop=True))
                                MM(fns, [Usb] + Btok + Ktok + Vtok, [hps])
                                V(lambda e, hps=hps: e.tensor_tensor(out=H32[:].rearrange("p h v -> p (h v)"),
                                                                     in0=Hd[:].rearrange("p h v -> p (h v)"),
                                                                     in1=hps[:, 0:NP * 64], op=ALU.add), [Hd, hps], [H32])
                                A(lambda e: e.activation(out=HbE[0:64, :, :], in_=H32[0:64, :, :], func=AF.Copy), [H32], [HbE])
                                A(lambda e: e.activation(out=HbO[64:128, :, :], in_=H32[64:128, :, :], func=AF.Copy),
                                  [H32], [HbO])
                            sq_ = [KH, BH, VB, sqb]
                            t1_ = [f["t1"], f["t2"], f["t3"], f["g"]]
                            yo_ = RT
                            PR = range(NP)
                            acc_ = {}
                            for pi in PR:
                                A(lambda e, pi=pi: e.activation(out=sq_[pi][:], in_=Yt[pi][:], func=AF.Copy), [Yt[pi]], [sq_[pi]])
                            for pi in PR:
                                a = npp()
                                acc_[pi] = a
                                MM([lambda e, a=a, pi=pi: e.matmul(a[:], lhsT=blkm, rhs=sq_[pi][:], start=True, stop=True)],
                                   [matb, sq_[pi]], [a])
                            for pi in PR:
                                V(lambda e, pi=pi, a=acc_[pi]: e.tensor_tensor(out=Yt[pi][:], in0=Yt[pi][:], in1=a[:], op=ALU.subtract),
                                  [Yt[pi], acc_[pi]], [Yt[pi]])
                            for pi in PR:
                                A(lambda e, pi=pi: e.activation(out=sq_[pi][:], in_=Yt[pi][:], func=AF.Square), [Yt[pi]], [sq_[pi]])
                            for pi in PR:
                                a = npp()
                                acc_[pi] = a
                                MM([lambda e, a=a, pi=pi: e.matmul(a[:], lhsT=blkm, rhs=sq_[pi][:], start=True, stop=True)],
                                   [matb, sq_[pi]], [a])
                            for pi in PR:
                                A(lambda e, pi=pi, a=acc_[pi]: e.activation(out=t1_[pi][:], in_=a[:], func=AF.Ln, bias=64e-5, scale=1.0),
                                  [acc_[pi]], [t1_[pi]])
                            for pi in PR:
                                A(lambda e, pi=pi: e.activation(out=t1_[pi][:], in_=t1_[pi][:], func=AF.Exp, scale=-0.5),
                                  [t1_[pi]], [t1_[pi]])
                            for pi in PR:
                                V(lambda e, pi=pi: e.tensor_tensor(out=Yt[pi][:], in0=Yt[pi][:], in1=t1_[pi][:], op=ALU.mult),
                                  [Yt[pi], t1_[pi]], [Yt[pi]])
                            for pi in PR:
                                pr = bt * NP + pi
                                V(lambda e, pi=pi, pr=pr: e.tensor_scalar(out=Yt[pi][:], in0=Yt[pi][:], scalar1=pvc(L, 160 + pr),
                                                                          scalar2=pvc(L, 168 + pr), op0=ALU.mult, op1=ALU.add),
                                  [Yt[pi], pv], [Yt[pi]])
                            for pi in PR:
                                V(lambda e, pi=pi: e.tensor_tensor(out=Yt[pi][:], in0=Yt[pi][:], in1=bonus[pi][:], op=ALU.add),
                                  [Yt[pi], bonus[pi]], [Yt[pi]])
                            for pi in PR:
                                ro = (bt * NP + pi) * 128
                                a = npp()
                                acc_[pi] = a
                                MM([lambda e, a=a, ro=ro: e.matmul(a[:], lhsT=wg0[:, ro:ro + 128], rhs=sg0[:], start=True, stop=False),
                                    lambda e, a=a, ro=ro: e.matmul(a[:], lhsT=wg1[0:32, ro:ro + 128], rhs=sg1[0:32, :],
                                                                   start=False, stop=True)], [wg0, wg1, sg0, sg1], [a])
                            for pi in PR:
                                ro = (bt * NP + pi) * 128
                                V(lambda e, pi=pi, a=acc_[pi]: e.tensor_tensor(out=yo_[pi][:], in0=Yt[pi][:], in1=a[:], op=ALU.mult),
                                  [Yt[pi], acc_[pi]], [yo_[pi]])
                                S.dma("sync", [(yT[ro:ro + 128, t0:t0 + 512], yo_[pi][:])], [yo_[pi].r], [R_yT])
                S.barrier()
                S.release(scope_res.pop(id(st), []))
            with ExitStack() as st:
                ulin2 = [sb(st, "ulin%d" % i, [128, 512], F32) for i in range(2)]
                ugat2 = [sb(st, "ugat%d" % i, [128, 512], F32) for i in range(2)]
                ulh = sb(st, "ulh", [128, 32], F32)
                ugh = sb(st, "ugh", [128, 32], F32)
                ub = sb(st, "ub", [128, 544], BF16)
                dg = sb(st, "dg", [128, 8, 31, 128], BF16)
                cT = sb(st, "cT", [128, 8, 512], F32)
                cb = sb(st, "cb", [128, 8, 512], BF16)
                tq2 = [sb(st, "tq%d" % i, [128, 512], F32) for i in range(2)]
                mean = sb(st, "mean", [128, 512], F32)
                rs = sb(st, "rs", [128, 512], F32)
                yo2 = [sb(st, "yoc%d" % i, [128, 512], BF16) for i in range(2)]
                pcv = [ps(st, "pcv%d" % i, [128, 512]) for i in range(4)]
                pst = [ps(st, "pst%d" % i, [128, 512]) for i in range(2)]
                ccnt = {"p": 0}
                for ch in range(8):
                    for j in range(31):
                        V(lambda e, j=j, ch=ch: e.tensor_scalar(out=dg[:, ch, j, :], in0=matb[:, 0:128],
                                                                scalar1=pvc(L, 200 + j * 8 + ch), scalar2=None,
                                                                op0=ALU.mult), [matb, pv], [dg], append=((ch, j) != (0, 0)))
                for b in range(NSEQ):
                    for seg in range(NSEG):
                        emit_casts(n=30)
                        t0 = b * T + seg * 512
                        halo = 30 if seg > 0 else 0
                        for ch in range(8):
                            r0 = RC + ch * 128
                            V(lambda e: e.memset(ub[:, 0:32], 0.0), [], [ub])
                            ulin, ugat = ulin2[ch % 2], ugat2[ch % 2]
                            if halo:
                                src0 = seg * 512 - halo
                                S.dma("sync", [(ulh[:, 0:halo], zT[r0:r0 + 128, b, 1 + src0:1 + src0 + halo])], [R_zT], [ulh.r])
                                S.dma("sync", [(ugh[:, 0:halo], zT[r0 + DC:r0 + DC + 128, b, 1 + src0:1 + src0 + halo])], [R_zT], [ugh.r])
                                A(lambda e: e.activation(out=ugh[:, 0:30], in_=ugh[:, 0:30], func=AF.Sigmoid), [ugh], [ugh])
                                V(lambda e: e.tensor_tensor(out=ub[:, 2:32], in0=ulh[:, 0:30], in1=ugh[:, 0:30], op=ALU.mult), [ulh, ugh], [ub])
                            src0 = seg * 512
                            S.dma("sync", [(ulin[:], zT[r0:r0 + 128, b, 1 + src0:1 + src0 + 512])], [R_zT], [ulin.r])
                            S.dma("sync", [(ugat[:], zT[r0 + DC:r0 + DC + 128, b, 1 + src0:1 + src0 + 512])], [R_zT], [ugat.r])
                            A(lambda e, ugat=ugat: e.activation(out=ugat[:], in_=ugat[:], func=AF.Sigmoid), [ugat], [ugat])
                            V(lambda e, ulin=ulin, ugat=ugat: e.tensor_tensor(out=ub[:, 32:544], in0=ulin[:], in1=ugat[:], op=ALU.mult),
                              [ulin, ugat], [ub])
                            a = pcv[ccnt["p"] % 4]
                            ccnt["p"] += 1
                            MM([(lambda e, j=j, a=a, ch=ch: e.matmul(a[:], lhsT=dg[:, ch, j, :], rhs=ub[:, 2 + j:2 + j + 512],
                                                              start=(j == 0), stop=(j == 30))) for j in range(31)],
                               [dg, ub], [a])
                            A(lambda e, a=a, ch=ch: e.activation(out=cT[:, ch, :], in_=a[:], func=AF.Identity,
                                                                 bias=pvc(L, 176 + ch), scale=1.0), [a, pv], [cT])
                        A(lambda e: e.activation(out=cb[:], in_=cT[:], func=AF.Copy), [cT], [cb])
                        MM([(lambda e, ch=ch: e.matmul(pst[0][:], lhsT=onesC, rhs=cb[:, ch, :], start=(ch == 0), stop=(ch == 7)))
                            for ch in range(8)], [cb, matb], [pst[0]])
                        V(lambda e: e.tensor_copy(out=mean[:], in_=pst[0][:]), [pst[0]], [mean])
                        for ch in range(8):
                            V(lambda e, ch=ch: e.tensor_tensor(out=cT[:, ch, :], in0=cT[:, ch, :], in1=mean[:], op=ALU.subtract),
                              [cT, mean], [cT])
                        A(lambda e: e.activation(out=cb[:], in_=cT[:], func=AF.Square), [cT], [cb])
                        MM([(lambda e, ch=ch: e.matmul(pst[1][:], lhsT=onesC, rhs=cb[:, ch, :], start=(ch == 0), stop=(ch == 7)))
                            for ch in range(8)], [cb, matb], [pst[1]])
                        A(lambda e: e.activation(out=rs[:], in_=pst[1][:], func=AF.Ln, bias=1e-5, scale=1.0), [pst[1]], [rs])
                        A(lambda e: e.activation(out=rs[:], in_=rs[:], func=AF.Exp, scale=-0.5), [rs], [rs])
                        for ch in range(8):
                            tq, yo = tq2[ch % 2], yo2[ch % 2]
                            V(lambda e, ch=ch, tq=tq: e.tensor_tensor(out=tq[:], in0=cT[:, ch, :], in1=rs[:], op=ALU.mult), [cT, rs], [tq])
                            V(lambda e, ch=ch, tq=tq: e.tensor_scalar(out=tq[:], in0=tq[:], scalar1=pvc(L, 184 + ch),
                                                                      scalar2=pvc(L, 192 + ch), op0=ALU.mult, op1=ALU.add), [tq, pv], [tq])
                            A(lambda e, tq=tq, yo=yo: e.activation(out=yo[:], in_=tq[:], func=AF.Silu), [tq], [yo])
                            S.dma("sync", [(yT[DR + ch * 128:DR + (ch + 1) * 128, t0:t0 + 512], yo[:])], [yo.r], [R_yT])
                S.barrier()
                S.release(scope_res.pop(id(st), []))

        for L in range(DEPTH + 1):
            tok_stage(L)
            if L < DEPTH:
                seq_stage(L)
        S.barrier()
        build.ninst = S.ninst
        build.nsem = S.nsem
    return nc


def _col(v, n):
    return np.ascontiguousarray(np.asarray(v, np.float32).reshape(n, 128).T)


def _pack_pv(inp, DEPTH):
    pv = np.zeros((DEPTH, 128, NPV), np.float32)
    for l in range(DEPTH):
        P = pv[l]
        for i, nm in enumerate(("norm_mix_pre", "norm_mix_post", "norm_mlp_pre", "norm_mlp_post", "norm_ple")):
            P[:, 16 * i:16 * i + 16] = _col(inp[nm][l], 16)
        mu = np.asarray(inp["mu_shift"][l], np.float32)
        for j in range(3):
            P[:, 80 + 8 * j:88 + 8 * j] = _col(mu[j * DR:(j + 1) * DR], 8)
        P[0:64, 104] = mu[3072:3136]
        P[0:64, 105] = mu[3136:3200]
        P[0:128, 106] = mu[3200:3328]
        P[0:32, 107] = mu[3328:3360]
        if l > 0:
            P[0:32, 108] = np.asarray(inp["mu_shift_vmix"][l - 1], np.float32)
            P[:, 128:136] = _col(inp["v0"][l - 1], 8)
        for i, nm in enumerate(("w0", "a0", None, "k_k", "k_a", "r_k", "gn_gain", "gn_bias")):
            if nm is not None:
                P[:, 112 + 8 * i:120 + 8 * i] = _col(np.asarray(inp[nm][l]).reshape(-1), 8)
        for i, nm in enumerate(("dw_b", "conv_ln_gain", "conv_ln_bias")):
            P[:, 176 + 8 * i:184 + 8 * i] = _col(inp[nm][l], 8)
        dw = np.asarray(inp["dw_w"][l], np.float32)
        for j in range(31):
            P[:, 200 + 8 * j:208 + 8 * j] = _col(dw[j], 8)
    return pv


def _consts():
    c = np.zeros((128, NCST), np.float32)
    c[:, 0:128] = np.eye(128)
    c[:, 128:256] = 1.0 / 2048
    c[0:64, 256:320] = 1.0
    c[64:128, 320:384] = 1.0
    c[:, 384:512] = 1.0 / 1024
    c[0:64, 512:576] = 1.0 / 64
    c[64:128, 576:640] = 1.0 / 64
    i = np.arange(128)
    st_s = (i[:, None] < i[None, :]).astype(np.float32)
    st_i = (i[:, None] <= i[None, :]).astype(np.float32)
    ts_s = (i[None, :] < i[:, None]).astype(np.float32)
    c[:, 640:1152] = np.tile(st_s, (1, 4))
    c[:, 1152:1664] = np.tile(st_i, (1, 4))
    c[:, 1664:2176] = np.tile(ts_s, (1, 4))
    c[:, 2176:2304] = 1.0
    c[:, 2304:2816] = np.tile(np.eye(128, dtype=np.float32), (1, 4))
    return c


def _levels():
    i = np.arange(128)
    out = np.zeros((128, 14 * 512), np.float32)
    for k in range(7):
        b = 1 << k
        t, s_ = i[:, None], i[None, :]
        off = ((t // (2 * b)) == (s_ // (2 * b))) & ((t % (2 * b)) >= b) & ((s_ % (2 * b)) < b)
        off = off.astype(np.float32)
        out[:, (2 * k) * 512:(2 * k + 1) * 512] = np.tile(off, (1, 4))
        out[:, (2 * k + 1) * 512:(2 * k + 2) * 512] = np.tile(off.T, (1, 4))
    return out


_CACHE = {}


def run(inputs, DEPTH, NSEQ, T, ncores):
    key = (DEPTH, NSEQ, T)
    if key not in _CACHE:
        _CACHE[key] = build(DEPTH, NSEQ, T)
    nc = _CACHE[key]
    f32 = lambda a: np.ascontiguousarray(np.asarray(a, np.float32))
    x = f32(inputs["x"])
    p = f32(inputs["p"])
    LV = max(DEPTH - 1, 1)
    shared = {
        "w_in": f32(inputs["w_in"]), "w_out": f32(inputs["w_out"]), "w_up": f32(inputs["w_up"]),
        "w_down": f32(inputs["w_down"]), "w_ple": f32(inputs["w_ple"]), "w_ple_gate": f32(inputs["w_ple_gate"]),
        "w_decay_up": f32(inputs["w_decay_up"]), "w_iclr_up": f32(inputs["w_iclr_up"]),
        "w_gate_up": f32(inputs["w_gate_up"]),
        "pv": _pack_pv(inputs, DEPTH), "cst": _consts(), "lvl": _levels(),
    }
    wv = np.zeros((LV, D, 32), np.float32)
    wu = np.zeros((LV, 32, DR), np.float32)
    if DEPTH > 1:
        wv[:] = f32(inputs["w_in_vmix"])[:DEPTH - 1]
        wu[:] = f32(inputs["w_vmix_up"])[:DEPTH - 1]
    shared["w_in_vmix"] = wv
    shared["w_vmix_up"] = wu
    in_maps = []
    for c in range(ncores):
        xs = x[c * NSEQ:(c + 1) * NSEQ].reshape(NSEQ * T, D)
        ps_ = p[:, c * NSEQ:(c + 1) * NSEQ].reshape(DEPTH, NSEQ * T, DPLE)
        m = dict(shared)
        m["xT"] = np.ascontiguousarray(xs.T)
        m["pT"] = np.ascontiguousarray(ps_.transpose(0, 2, 1))
        in_maps.append(m)
    res = run_bass_kernel_spmd(nc, in_maps, core_ids=list(range(ncores)))
    outs = [np.asarray(r["outT"]).T.reshape(NSEQ, T, D) for r in res.results]
    return np.ascontiguousarray(np.concatenate(outs, axis=0).astype(np.float32))


def kernel(**inputs):
    return run(inputs, 4, 2, 2048, 8)
```

```python
import numpy as np
from contextlib import ExitStack
import concourse.bass as bass
import concourse.mybir as mybir
from concourse.bass_utils import run_bass_kernel_spmd

F32 = mybir.dt.float32
BF16 = mybir.dt.bfloat16
ALU = mybir.AluOpType
AF = mybir.ActivationFunctionType

D = 2048
DR = 1024
DC = 1024
RC = 3360
DIN = 5408
DZ = 5440
DFF = 8192
DPLE = 256
NPV = 448
NCST = 5 * 128 + 3 * 512 + 128 + 512
SEM_LIMIT = 30000


class Res:
    __slots__ = ("name", "w", "r", "dsem", "const")

    def __init__(self, name):
        self.name = name
        self.w = []
        self.r = {}
        self.dsem = None
        self.const = False


class Tile:
    def __init__(self, t, r):
        self.t = t
        self.r = r

    def __getitem__(self, idx):
        return self.t[idx]


class Sched:
    ENGS = ("sync", "gpsimd", "scalar", "vector", "tensor")

    def __init__(self, nc, stack):
        self.nc = nc
        self.stack = stack
        self.eng = {"sync": nc.sync, "gpsimd": nc.gpsimd, "scalar": nc.scalar,
                    "vector": nc.vector, "tensor": nc.tensor}
        self.nsem = 0
        self.cur = {}
        self.seen = {e: {} for e in self.ENGS}
        self.dsems = []
        self.ninst = 0
        self.rec = None
        self.free = []
        for e in self.ENGS:
            self.cur[e] = self._newsem()

    def _newsem(self):
        s = self.stack.enter_context(self.nc.semaphore("s%d" % self.nsem))
        self.nsem += 1
        return [s, self.nsem, 0]

    def _deps(self, e, reads, writes, append=False):
        need = {}

        def add(ev, same_ok):
            sem, key, val = ev
            if key == self.cur[e][1] and not same_ok:
                return
            if self.seen[e].get(key, 0) >= val:
                return
            if key not in need or need[key][2] < val:
                need[key] = ev

        same_raw = e in ("scalar", "vector", "gpsimd")
        for r in reads:
            for ev in r.w:
                add(ev, same_raw)
        for w in writes:
            if not append:
                for ev in w.w:
                    add(ev, same_raw)
            for ev in w.r.values():
                add(ev, False)
        E = self.eng[e]
        for ev in need.values():
            E.wait_ge(ev[0], ev[2])
            self.seen[e][ev[1]] = ev[2]
            self.ninst += 1

    def _mark(self, ev, reads, writes, append=False):
        for r in reads:
            if not r.const:
                old = r.r.get(ev[1])
                if old is None or old[2] < ev[2]:
                    r.r[ev[1]] = ev
        for w in writes:
            if append:
                w.w = w.w + [ev]
            else:
                w.w = [ev]
                w.r = {}

    def _tick(self, e):
        c = self.cur[e]
        if c[2] >= SEM_LIMIT:
            c = self._newsem()
            self.cur[e] = c
        c[2] += 1
        return (c[0], c[1], c[2])

    def op(self, e, fn, reads=(), writes=(), append=False):
        if self.rec is not None:
            self.rec.append((self.op, (e, fn, reads, writes, append), {}))
            return
        self._deps(e, reads, writes, append)
        ev = self._tick(e)
        fn(self.eng[e]).then_inc(ev[0], 1)
        self.ninst += 1
        self._mark(ev, reads, writes, append)

    def mm(self, fns, reads=(), writes=()):
        if self.rec is not None:
            self.rec.append((self.mm, (fns, reads, writes), {}))
            return
        self._deps("tensor", reads, writes)
        ev = self._tick("tensor")
        n = len(fns)
        for i, fn in enumerate(fns):
            ins = fn(self.nc.tensor)
            if i == n - 1:
                ins.then_inc(ev[0], 1)
        self.ninst += n
        self._mark(ev, reads, writes)

    def dma(self, e, pairs, reads=(), writes=(), **kw):
        if self.rec is not None:
            self.rec.append((self.dma, (e, pairs, reads, writes), kw))
            return
        self._deps(e, reads, writes)
        w0 = writes[0]
        if w0.dsem is None or w0.dsem[2] + 16 * len(pairs) > SEM_LIMIT:
            old = w0.dsem
            if old is None and self.free and self.free[-1][2] + 16 * len(pairs) <= SEM_LIMIT:
                w0.dsem = self.free.pop()
            else:
                w0.dsem = self._newsem()
                self.dsems.append(w0.dsem)
            keep = [(old[0], old[1], old[2])] if old is not None and old[2] > 0 else []
        else:
            keep = []
        ds = w0.dsem
        for (o, i) in pairs:
            self.eng[e].dma_start(out=o, in_=i, **kw).then_inc(ds[0], 16)
            ds[2] += 16
            self.ninst += 1
        ev = (ds[0], ds[1], ds[2])
        for r in reads:
            if not r.const:
                r.r[ev[1]] = ev
        for w in writes:
            w.w = keep + [ev]
            w.r = {}

    def replay_merge(self, A, B):
        assert self.rec is None
        na, nb = len(A), len(B)
        ia = ib = 0
        while ia < na or ib < nb:
            if ib >= nb or (ia < na and ia * nb <= ib * na):
                f, a, k = A[ia]
                ia += 1
            else:
                f, a, k = B[ib]
                ib += 1
            f(*a, **k)

    def release(self, res_list):
        for r in res_list:
            if r.dsem is not None:
                self.free.append(r.dsem)
                r.dsem = None

    def barrier(self):
        evs = []
        for e in self.ENGS:
            c = self.cur[e]
            if c[2] > 0:
                evs.append((c[0], c[1], c[2]))
        for d in self.dsems:
            if d[2] > 0:
                evs.append((d[0], d[1], d[2]))
        for e in self.ENGS:
            for ev in evs:
                if self.seen[e].get(ev[1], 0) < ev[2]:
                    self.eng[e].wait_ge(ev[0], ev[2])
                    self.seen[e][ev[1]] = ev[2]
                    self.ninst += 1


def build(DEPTH, NSEQ, T):
    TOK = NSEQ * T
    NSEG = T // 512
    NT = TOK // 512
    LV = max(DEPTH - 1, 1)
    nc = bass.Bass("TRN2", target_bir_lowering=False)

    def din(name, shape, dt=F32):
        return nc.dram_tensor(name, list(shape), dt, kind="ExternalInput").ap()

    def dscr(name, shape, dt):
        return nc.dram_tensor(name, list(shape), dt, kind="Internal").ap()

    xT_in = din("xT", [D, TOK])
    pT_in = din("pT", [DEPTH, DPLE, TOK])
    w_in = din("w_in", [DEPTH, D, DIN])
    w_vm = din("w_in_vmix", [LV, D, 32])
    w_out = din("w_out", [DEPTH, D, D])
    w_up = din("w_up", [DEPTH, D, DFF])
    w_dn = din("w_down", [DEPTH, DFF, D])
    w_ple = din("w_ple", [DEPTH, DPLE, D])
    w_pg = din("w_ple_gate", [DEPTH, D, D])
    w_du = din("w_decay_up", [DEPTH, 64, DR])
    w_iu = din("w_iclr_up", [DEPTH, 64, DR])
    w_vu = din("w_vmix_up", [LV, 32, DR])
    w_gu = din("w_gate_up", [DEPTH, 160, DR])
    pv_in = din("pv", [DEPTH, 128, NPV])
    cst_in = din("cst", [128, NCST])
    lvl_in = din("lvl", [128, 14 * 512])
    outT = nc.dram_tensor("outT", [D, TOK], F32, kind="ExternalOutput").ap()

    xT = dscr("xTs", [D, TOK], F32)
    zT = dscr("zTs", [DZ, NSEQ, T + 1], F32)
    yT = dscr("yTs", [D, TOK], BF16)
    vfT = dscr("vfTs", [DR, TOK], F32)
    Wb_in = dscr("Wb_in", [DEPTH, D, DZ], BF16)
    Wb_out = dscr("Wb_out", [DEPTH, D, D], BF16)
    Wb_up = dscr("Wb_up", [DEPTH, D, DFF], BF16)
    Wb_dn = dscr("Wb_dn", [DEPTH, DFF, D], BF16)
    Wb_ple = dscr("Wb_ple", [DEPTH, DPLE, D], BF16)
    Wb_pg = dscr("Wb_pg", [DEPTH, D, D], BF16)

    with ExitStack() as top:
        S = Sched(nc, top)
        R_xT, R_zT, R_yT, R_vf, R_W, R_out = (Res(n) for n in ("xT", "zT", "yT", "vf", "W", "out"))
        R_in = Res("inputs")
        R_in.const = True
        RW = {}

        uid = [0]
        scope_res = {}

        def sb(stack, name, shape, dt):
            uid[0] += 1
            name = "%s_u%d" % (name, uid[0])
            t = stack.enter_context(nc.sbuf_tensor(name, list(shape), dt))
            r = Res(name)
            scope_res.setdefault(id(stack), []).append(r)
            return Tile(t, r)

        def ps(stack, name, shape, dt=F32):
            uid[0] += 1
            name = "%s_u%d" % (name, uid[0])
            t = stack.enter_context(nc.psum_tensor(name, list(shape), dt))
            return Tile(t, Res(name))

        V = lambda fn, reads, writes, append=False: S.op("vector", fn, [x.r for x in reads], [x.r for x in writes], append)
        A = lambda fn, reads, writes: S.op("scalar", fn, [x.r for x in reads], [x.r for x in writes])
        G_ = lambda fn, reads, writes: S.op("gpsimd", fn, [x.r for x in reads], [x.r for x in writes])
        MM = lambda fns, reads, writes: S.mm(fns, [x.r for x in reads], [x.r for x in writes])

        cst = sb(top, "cst", [128, NCST], F32)
        pv = sb(top, "pv", [128, DEPTH, NPV], F32)
        matb = sb(top, "matb", [128, 5 * 128], BF16)
        S.dma("sync", [(cst[:], cst_in[:, :])], [R_in], [cst.r])
        lvlm = sb(top, "lvlm", [128, 14 * 512], BF16)
        S.dma("gpsimd", [(lvlm[:], lvl_in[:, :])], [R_in], [lvlm.r])
        lvlm.r.const = True
        S.dma("sync", [(pv[:, l, :], pv_in[l, :, :]) for l in range(DEPTH)], [R_in], [pv.r])
        V(lambda e: e.tensor_copy(out=matb[:], in_=cst[:, 0:640]), [cst], [matb])
        cst.r.const = True
        pv.r.const = True
        matb.r.const = True
        ident = matb[:, 0:128]
        onesD = matb[:, 128:256]
        blk1 = matb[:, 256:384]
        onesC = matb[:, 384:512]
        blkm = matb[:, 512:640]
        mS_st = cst[:, 640:1152]
        mI_st = cst[:, 1152:1664]
        mS_ts = cst[:, 1664:2176]
        ones_f = cst[:, 2176:2304]
        identx4 = cst[:, 2304:2816]

        def pvc(l, c, n=1, p=128):
            return pv[0:p, l, c:c + n]

        with ExitStack() as st0:
            zz = sb(st0, "zz", [128, 8], F32)
            V(lambda e: e.memset(zz[:], 0.0), [], [zz])
            prs = []
            for r0 in range(0, DZ, 128):
                n = min(128, DZ - r0)
                for b in range(NSEQ):
                    prs.append((zT[r0:r0 + n, b, 0:1], zz[0:n, 0:1]))
            S.dma("sync", prs, [zz.r], [R_zT], allow_slow_non_contiguous=True)

            casts = []

            def add_casts(grp, l, kinds):
                for kind in kinds:
                    key = (kind, l)
                    RW[key] = Res("W_%s_%d" % key)
                    if kind == "in":
                        for k0 in range(0, D, 128):
                            ks = slice(k0, k0 + 128)
                            casts.append((grp, key, Wb_in[l, ks, 0:DIN], w_in[l, ks, :]))
                            if l > 0:
                                casts.append((grp, key, Wb_in[l, ks, DIN:DZ], w_vm[l - 1, ks, :]))
                    elif kind == "out":
                        for k0 in range(0, D, 128):
                            casts.append((grp, key, Wb_out[l, k0:k0 + 128, :], w_out[l, k0:k0 + 128, :]))
                    elif kind == "pg":
                        for k0 in range(0, D, 128):
                            casts.append((grp, key, Wb_pg[l, k0:k0 + 128, :], w_pg[l, k0:k0 + 128, :]))
                    elif kind == "up":
                        for k0 in range(0, D, 128):
                            for c0 in range(0, DFF, 2048):
                                casts.append((grp, key, Wb_up[l, k0:k0 + 128, c0:c0 + 2048], w_up[l, k0:k0 + 128, c0:c0 + 2048]))
                    elif kind == "dn":
                        for k0 in range(0, DFF, 256):
                            casts.append((grp, key, Wb_dn[l, k0:k0 + 256, :], w_dn[l, k0:k0 + 256, :]))
                    elif kind == "ple":
                        casts.append((grp, key, Wb_ple[l, :, :], w_ple[l, :, :]))

            add_casts(0, 0, ["in"])
            for LL in range(1, DEPTH + 1):
                add_casts(LL, LL - 1, ["out", "up", "dn", "ple", "pg"])
                if LL < DEPTH:
                    add_casts(LL, LL, ["in"])
            cpos = [0]

            def emit_casts(n=None, grp=None):
                while cpos[0] < len(casts):
                    g, key, o, i = casts[cpos[0]]
                    if grp is not None:
                        if g > grp:
                            break
                    elif n is not None:
                        if n <= 0:
                            break
                        n -= 1
                    S.dma("gpsimd", [(o, i)], [R_in], [RW[key]])
                    cpos[0] += 1

            emit_casts(grp=0)
            S.barrier()

        def tok_stage(L):
            fin = L > 0
            start = L < DEPTH
            l = L - 1
            emit_casts(grp=L)
            with ExitStack() as st:
                xt = sb(st, "xt", [128, 16, 512], F32)
                hT = sb(st, "hT", [128, 16, 512], BF16)
                uT = sb(st, "uT", [128, 32, 512], BF16)
                stg = sb(st, "stg", [128, 16, 512], F32)
                slabs = [sb(st, "slab%d" % i, [128, 16, 512], BF16) for i in range(3)]
                rstd = sb(st, "rstd", [128, 512], F32)
                tmpf = [sb(st, "tmpf%d" % i, [128, 512], F32) for i in range(3)]
                pTb = sb(st, "pTb", [128, 2, 512], BF16)
                acc = [ps(st, "acc%d" % i, [128, 512]) for i in range(6)]
                ssum = ps(st, "ssum", [128, 512])
                cnt = {"slab": 0, "acc": 0, "tmp": 0}

                plan = []
                for _ti in range(NT):
                    if fin:
                        for c0 in range(0, D, 512):
                            plan.append((("out", l), Wb_out[l], 0, c0, 512, 16))
                        for hh in range(2):
                            for c0 in range(0, 4096, 512):
                                plan.append((("up", l), Wb_up[l], 0, hh * 4096 + c0, 512, 16))
                            for c0 in range(0, D, 512):
                                for kq in range(2):
                                    plan.append((("dn", l), Wb_dn[l], hh * 4096 + kq * 2048, c0, 512, 16))
                        for c0 in range(0, D, 512):
                            plan.append((("ple", l), Wb_ple[l], 0, c0, 512, 2))
                        for c0 in range(0, D, 512):
                            plan.append((("pg", l), Wb_pg[l], 0, c0, 512, 16))
                    if start:
                        MZ_ = DZ if L > 0 else DIN
                        for c0 in range(0, MZ_, 512):
                            plan.append((("in", L), Wb_in[L], 0, c0, min(512, MZ_ - c0), 16))
                ppos = {"use": 0, "ld": 0}

                def issue_upto(n):
                    while ppos["ld"] < min(n, len(plan)):
                        key, W, k0, c0, w, kc = plan[ppos["ld"]]
                        sl = slabs[ppos["ld"] % 3]
                        src = W[k0:k0 + kc * 128, c0:c0 + w].rearrange("(c p) m -> p c m", p=128)
                        S.dma("sync", [(sl[:, 0:kc, 0:w], src)], [RW[key]], [sl.r])
                        ppos["ld"] += 1

                def load_slab(key, k0, c0, w, kc=16):
                    i = ppos["use"]
                    pk, W, pk0, pc0, pw, pkc = plan[i]
                    assert (pk, pk0, pc0, pw, pkc) == (key, k0, c0, w, kc), (plan[i][0], plan[i][2:], key, k0, c0, w, kc)
                    issue_upto(i + 3)
                    ppos["use"] += 1
                    return slabs[i % 3]

                def next_acc():
                    a = acc[cnt["acc"] % 6]
                    cnt["acc"] += 1
                    return a

                def next_tmp():
                    a = tmpf[cnt["tmp"] % 3]
                    cnt["tmp"] += 1
                    return a

                def norm_stats(src, scr=None, off=0):
                    scr = hT if scr is None else scr
                    A(lambda e: e.activation(out=scr[:, off:off + 9, :], in_=src[:, 0:9, :], func=AF.Square), [src], [scr])
                    V(lambda e: e.tensor_tensor(out=scr[:, off + 9:off + 16, :], in0=src[:, 9:16, :], in1=src[:, 9:16, :], op=ALU.mult),
                      [src], [scr], append=True)
                    MM([(lambda e, c=c: e.matmul(ssum[:], lhsT=onesD, rhs=scr[:, off + c, :],
                                                 start=(c == 0), stop=(c == 15))) for c in range(16)],
                       [scr, matb], [ssum])
                    A(lambda e: e.activation(out=rstd[:], in_=ssum[:], func=AF.Ln, bias=1e-6, scale=1.0),
                      [ssum], [rstd])
                    A(lambda e: e.activation(out=rstd[:], in_=rstd[:], func=AF.Exp, scale=-0.5), [rstd], [rstd])

                def scale_by(dst, src, gcol, ll):
                    for c in range(16):
                        V(lambda e, c=c: e.scalar_tensor_tensor(
                            out=dst[:, c, :], in0=src[:, c, :], scalar=pvc(ll, gcol + c), in1=rstd[:],
                            op0=ALU.mult, op1=ALU.mult), [src, rstd, pv], [dst])

                def add_into_x(src):
                    V(lambda e: e.tensor_tensor(out=xt[:], in0=xt[:], in1=src[:], op=ALU.add), [xt, src], [xt])

                def dense(key, rhsT, KC, M, evac, cbase=0):
                    for c0 in range(0, M, 512):
                        w = min(512, M - c0)
                        sl = load_slab(key, 0, cbase + c0, w, kc=KC)
                        for m0 in range(0, w, 128):
                            msz = min(128, w - m0)
                            a = next_acc()
                            MM([(lambda e, c=c, a=a, m0=m0, msz=msz, sl=sl: e.matmul(
                                a[0:msz, :], lhsT=sl[:, c, m0:m0 + msz], rhs=rhsT[:, c, :],
                                start=(c == 0), stop=(c == KC - 1))) for c in range(KC)],
                               [sl, rhsT], [a])
                            evac((c0 + m0) // 128, msz, a)

                def load_y(tj):
                    ysrc = yT[:, tj * 512:tj * 512 + 512].rearrange("(c p) t -> p c t", p=128)
                    S.dma("sync", [(uT[:, 0:16, :], ysrc)], [R_yT], [uT.r])

                for ti in range(NT):
                    b = ti // NSEG
                    seg = ti % NSEG
                    t0 = ti * 512
                    xsrc = (xT_in if L == 0 else xT)[:, t0:t0 + 512].rearrange("(c p) t -> p c t", p=128)
                    S.dma("sync", [(xt[:], xsrc)], [R_in if L == 0 else R_xT], [xt.r])
                    if fin:
                        if ti == 0:
                            load_y(0)
                        for c in range(2):
                            tm = next_tmp()
                            S.dma("sync", [(tm[:], pT_in[l, c * 128:(c + 1) * 128, t0:t0 + 512])], [R_in], [tm.r])
                            V(lambda e, c=c, tm=tm: e.tensor_copy(out=pTb[:, c, :], in_=tm[:]), [tm], [pTb])

                        def ev_copy(mi, msz, a):
                            A(lambda e: e.activation(out=stg[0:msz, mi, :], in_=a[0:msz, :], func=AF.Copy),
                              [a], [stg])
                        dense(("out", l), uT, 16, D, ev_copy)
                        norm_stats(stg)
                        scale_by(stg, stg, 16, l)
                        add_into_x(stg)
                        norm_stats(xt)
                        scale_by(hT, xt, 32, l)
                        for hh in range(2):
                            def ev_relu2(mi, msz, a):
                                tm = next_tmp()
                                A(lambda e: e.activation(out=tm[:], in_=a[:], func=AF.Relu), [a], [tm])
                                V(lambda e: e.tensor_tensor(out=uT[:, mi, :], in0=tm[:], in1=tm[:], op=ALU.mult),
                                  [tm], [uT])
                            dense(("up", l), hT, 16, 4096, ev_relu2, cbase=hh * 4096)
                            for c0 in range(0, D, 512):
                                accs = [next_acc() for _ in range(4)]
                                for kq in range(2):
                                    sl = load_slab(("dn", l), hh * 4096 + kq * 2048, c0, 512)
                                    for mi in range(4):
                                        a = accs[mi]
                                        MM([(lambda e, c=c, a=a, mi=mi, sl=sl, kq=kq: e.matmul(
                                            a[:], lhsT=sl[:, c, mi * 128:(mi + 1) * 128], rhs=uT[:, kq * 16 + c, :],
                                            start=(kq == 0 and c == 0), stop=(kq == 1 and c == 15)))
                                            for c in range(16)], [sl, uT], [a])
                                for mi in range(4):
                                    a = accs[mi]
                                    m = c0 // 128 + mi
                                    if hh == 0:
                                        A(lambda e, a=a, m=m: e.activation(out=stg[:, m, :], in_=a[:], func=AF.Copy),
                                          [a], [stg])
                                    else:
                                        V(lambda e, a=a, m=m: e.tensor_tensor(out=stg[:, m, :], in0=stg[:, m, :],
                                                                              in1=a[:], op=ALU.add), [a, stg], [stg])
                        if ti + 1 < NT:
                            load_y(ti + 1)
                        norm_stats(stg)
                        scale_by(stg, stg, 48, l)
                        add_into_x(stg)
                        A(lambda e: e.activation(out=hT[:], in_=xt[:], func=AF.Copy), [xt], [hT])
                        dense(("ple", l), pTb, 2, D, ev_copy)
                        norm_stats(stg, uT, 16)
                        scale_by(stg, stg, 64, l)

                        def ev_gate(mi, msz, a):
                            tm = next_tmp()
                            A(lambda e: e.activation(out=tm[:], in_=a[:], func=AF.Sigmoid), [a], [tm])
                            V(lambda e: e.tensor_tensor(out=stg[:, mi, :], in0=stg[:, mi, :], in1=tm[:], op=ALU.mult),
                              [tm, stg], [stg])
                        dense(("pg", l), hT, 16, D, ev_gate)
                        add_into_x(stg)
                    if start:
                        norm_stats(xt)
                        scale_by(hT, xt, 0, L)
                        MZ = DZ if L > 0 else DIN

                        def ev_z(mi, msz, a):
                            tm = next_tmp()
                            A(lambda e: e.activation(out=tm[0:msz, :], in_=a[0:msz, :], func=AF.Copy), [a], [tm])
                            S.dma("sync", [(zT[mi * 128:mi * 128 + msz, b, 1 + seg * 512:1 + seg * 512 + 512],
                                              tm[0:msz, :])], [tm.r], [R_zT])
                        dense(("in", L), hT, 16, MZ, ev_z)
                    dst = (outT if L == DEPTH else xT)[:, t0:t0 + 512].rearrange("(c p) t -> p c t", p=128)
                    S.dma("sync", [(dst, xt[:])], [xt.r], [R_out if L == DEPTH else R_xT])
                    emit_casts(n=14)
                S.barrier()
                S.release(scope_res.pop(id(st), []))

        def seq_stage(L):
            with ExitStack() as st:
                wdu = sb(st, "wdu", [64, DR], BF16)
                wiu = sb(st, "wiu", [64, DR], BF16)
                wvu = sb(st, "wvu", [32, DR], BF16)
                wg0 = sb(st, "wg0", [128, DR], BF16)
                wg1 = sb(st, "wg1", [32, DR], BF16)
                lst = [(wdu, w_du[L], 64), (wiu, w_iu[L], 64), (wg0, w_gu[L, 0:128, :], 128),
                       (wg1, w_gu[L, 128:160, :], 32)]
                if L > 0:
                    lst.append((wvu, w_vu[L - 1], 32))
                lwst = ExitStack()
                lwf = sb(lwst, "lwf", [128, DR], F32)
                for (dstt, src, n) in lst:
                    S.dma("sync", [(lwf[0:n, :], src)], [R_in], [lwf.r])
                    V(lambda e, dstt=dstt, n=n: e.tensor_copy(out=dstt[0:n, :], in_=lwf[0:n, :]), [lwf], [dstt])
                S.barrier()
                S.release(scope_res.pop(id(lwst), []))
                lwst.close()
                sm_raw0 = sb(st, "sm_raw0", [128, 513], F32)
                sm_t = sb(st, "sm_t", [128, 512], F32)
                twd = sb(st, "twd", [64, 512], BF16)
                adb = sb(st, "adb", [64, 512], BF16)
                sg0 = sb(st, "sg0", [128, 512], BF16)
                sg1 = sb(st, "sg1", [32, 512], BF16)
                vdb = sb(st, "vdb", [32, 512], BF16)
                rawS = [[sb(st, "raw%d_%d" % (k, i), [128, 513], F32) for i in range(3)] for k in range(2)]
                f = {n: sb(st, "f_" + n, [128, 512], F32) for n in
                     ("r", "k", "v", "t1", "t2", "t3", "g", "eg", "kkn", "icl", "k2", "bb", "vf")}
                sqb = sb(st, "sqb", [128, 512], BF16)
                NP = 4
                RT = [sb(st, "RT%d" % i, [128, 512], BF16) for i in range(NP)]
                AT = [sb(st, "AT%d" % i, [128, 512], BF16) for i in range(NP)]
                BT = [sb(st, "BT%d" % i, [128, 512], BF16) for i in range(NP)]
                KT = [sb(st, "KT%d" % i, [128, 512], BF16) for i in range(NP)]
                KH = sb(st, "KH", [128, 512], BF16)
                BH = sb(st, "BH", [128, 512], BF16)
                VB = sb(st, "VB", [128, 512], BF16)
                Ktok = [sb(st, "Ktok%d" % i, [128, 4, 128], BF16) for i in range(NP)]
                Btok = [sb(st, "Btok%d" % i, [128, 4, 128], BF16) for i in range(NP)]
                Vtok = [sb(st, "Vtok%d" % i, [128, 4, 128], BF16) for i in range(NP)]
                bonus = [sb(st, "bonus%d" % i, [128, 512], F32) for i in range(NP)]
                egC = [sb(st, "egC%d" % i, [128, 4], F32) for i in range(NP)]
                Yt = [sb(st, "Yt%d" % i, [128, 512], F32) for i in range(NP)]
                AakT = [sb(st, "AakT%d" % i, [128, 512], BF16) for i in range(2 * NP)]
                ArbT = [sb(st, "ArbT%d" % i, [128, 512], BF16) for i in range(2 * NP)]
                ArkT = [sb(st, "ArkT%d" % i, [128, 512], BF16) for i in range(2 * NP)]
                TT = [sb(st, "TT%d" % i, [128, 512], BF16) for i in range(2 * NP)]
                Mh = [sb(st, "Mh%d" % i, [128, 512], BF16) for i in range(4)]
                Ph = [sb(st, "Ph%d" % i, [128, 512], BF16) for i in range(4)]
                Twh = [[sb(st, "Tw%d_%d" % (i, j), [128, 512], BF16) for j in range(2)] for i in range(4)]
                TTwh = [[sb(st, "TTw%d_%d" % (i, j), [128, 512], BF16) for j in range(2)] for i in range(4)]
                Zbh = [[sb(st, "Zb%d_%d" % (i, j), [128, 512], BF16) for j in range(2)] for i in range(4)]
                H32 = sb(st, "H32", [128, NP, 64], F32)
                Hd = sb(st, "Hd", [128, NP, 64], F32)
                HbE = sb(st, "HbE", [128, NP, 64], BF16)
                HbO = sb(st, "HbO", [128, NP, 64], BF16)
                Wsb = sb(st, "Wsb", [128, 2 * NP, 64], BF16)
                Usb = sb(st, "Usb", [128, 2 * NP, 64], BF16)
                pp = [ps(st, "pp%d" % i, [128, 512]) for i in range(5)]
                pq = [ps(st, "pq%d" % i, [128, 512]) for i in range(2)]
                ptr = ps(st, "ptr", [128, 8, 128], BF16)
                pc = {"p": 0, "q": 0}

                def npp():
                    a = pp[pc["p"] % 5]
                    pc["p"] += 1
                    return a

                def nq():
                    a = pq[pc["q"] % 2]
                    pc["q"] += 1
                    return a

                def rsqrt_to(dst, src_ps, scale, bias):
                    A(lambda e: e.activation(out=dst[:], in_=src_ps[:], func=AF.Ln, bias=bias, scale=scale),
                      [src_ps], [dst])
                    A(lambda e: e.activation(out=dst[:], in_=dst[:], func=AF.Exp, scale=-0.5), [dst], [dst])

                for b in range(NSEQ):
                    for bt in range(8 // NP):
                        V(lambda e: e.memset(H32[:], 0.0), [], [H32])
                        V(lambda e: e.memset(HbE[:], 0.0), [], [HbE])
                        V(lambda e: e.memset(HbO[:], 0.0), [], [HbO])
                        for seg in range(NSEG):
                            emit_casts(n=30)
                            c0 = seg * 512
                            t0 = b * T + seg * 512
                            smalls = [(3072, 64, 104, AF.Tanh, twd), (3136, 64, 105, AF.Copy, adb),
                                      (3200, 128, 106, AF.Sigmoid, sg0), (3328, 32, 107, AF.Sigmoid, sg1)]
                            if L > 0:
                                smalls.append((5408, 32, 108, AF.Copy, vdb))
                            for si_, (r0, n, mc, fn, dstt) in enumerate(smalls):
                                sm_raw = (sm_raw0, rawS[1][0], rawS[1][1])[si_ % 3]
                                S.dma("sync", [(sm_raw[0:n, :], zT[r0:r0 + n, b, c0:c0 + 513])], [R_zT], [sm_raw.r])
                                V(lambda e, n=n: e.tensor_tensor(out=sm_t[0:n, :], in0=sm_raw[0:n, 0:512],
                                                                 in1=sm_raw[0:n, 1:513], op=ALU.subtract),
                                  [sm_raw], [sm_t])
                                V(lambda e, n=n, mc=mc: e.scalar_tensor_tensor(
                                    out=sm_t[0:n, :], in0=sm_t[0:n, :], scalar=pvc(L, mc, 1, n), in1=sm_raw[0:n, 1:513],
                                    op0=ALU.mult, op1=ALU.add), [sm_t, sm_raw, pv], [sm_t])
                                A(lambda e, n=n, fn=fn, dstt=dstt: e.activation(out=dstt[0:n, :], in_=sm_t[0:n, :], func=fn),
                                  [sm_t], [dstt])
                            def load_raw(pj):
                                roj = (bt * NP + pj) * 128
                                for j in range(3):
                                    S.dma("sync", [(rawS[pj % 2][j][:], zT[j * DR + roj:j * DR + roj + 128, b, c0:c0 + 513])],
                                          [R_zT], [rawS[pj % 2][j].r])

                            def prep_pair(pi):
                                PO = G_ if L > 0 else V
                                pr = bt * NP + pi
                                ro = pr * 128
                                raw = rawS[pi % 2]
                                if pi == 0:
                                    load_raw(0)
                                if pi + 1 < NP:
                                    load_raw(pi + 1)
                                for j, nm in enumerate(("r", "k", "v")):
                                    V(lambda e, j=j: e.tensor_tensor(out=f["t1"][:], in0=raw[j][:, 0:512],
                                                                     in1=raw[j][:, 1:513], op=ALU.subtract),
                                      [raw[j]], [f["t1"]])
                                    V(lambda e, j=j, nm=nm: e.scalar_tensor_tensor(
                                        out=f[nm][:], in0=f["t1"][:], scalar=pvc(L, 80 + 8 * j + pr), in1=raw[j][:, 1:513],
                                        op0=ALU.mult, op1=ALU.add), [f["t1"], raw[j], pv], [f[nm]])
                                a = nq()
                                MM([lambda e, a=a: e.matmul(a[:], lhsT=wdu[0:64, ro:ro + 128], rhs=twd[0:64, :],
                                                            start=True, stop=True)], [wdu, twd], [a])
                                A(lambda e, a=a: e.activation(out=f["t1"][:], in_=a[:], func=AF.Sigmoid,
                                                              bias=pvc(L, 112 + pr), scale=1.0), [a, pv], [f["t1"]])
                                V(lambda e: e.tensor_scalar(out=f["t1"][:], in0=f["t1"][:], scalar1=-0.6065306597126334,
                                                            scalar2=None, op0=ALU.mult), [f["t1"]], [f["t1"]])
                                for c in range(4):
                                    V(lambda e, c=c: e.tensor_tensor_scan(
                                        out=f["g"][:, c * 128:(c + 1) * 128], data0=ones_f,
                                        data1=f["t1"][:, c * 128:(c + 1) * 128], initial=0.0,
                                        op0=ALU.mult, op1=ALU.add), [f["t1"], cst], [f["g"]])
                                A(lambda e: e.activation(out=f["eg"][:], in_=f["g"][:], func=AF.Exp), [f["g"]], [f["eg"]])
                                V(lambda e, pi=pi: e.tensor_copy(
                                    out=egC[pi][:], in_=f["eg"][:].rearrange("p (c t) -> p c t", t=128)[:, :, 127]),
                                  [f["eg"]], [egC[pi]])
                                V(lambda e, pi=pi: e.tensor_tensor(out=RT[pi][:], in0=f["r"][:], in1=f["eg"][:], op=ALU.mult),
                                  [f["r"], f["eg"]], [RT[pi]])
                                V(lambda e: e.tensor_tensor(out=f["t2"][:], in0=f["g"][:], in1=f["t1"][:], op=ALU.subtract),
                                  [f["g"], f["t1"]], [f["t2"]])
                                A(lambda e: e.activation(out=f["t2"][:], in_=f["t2"][:], func=AF.Exp), [f["t2"]], [f["t2"]])
                                A(lambda e: e.activation(out=f["t3"][:], in_=f["g"][:], func=AF.Exp, scale=-1.0),
                                  [f["g"]], [f["t3"]])
                                for c in range(4):
                                    V(lambda e, c=c: e.tensor_scalar(
                                        out=f["eg"][:, c * 128:(c + 1) * 128], in0=f["g"][:, c * 128:(c + 1) * 128],
                                        scalar1=f["g"][:, c * 128 + 127:c * 128 + 128], scalar2=-1.0,
                                        op0=ALU.subtract, op1=ALU.mult), [f["g"]], [f["eg"]])
                                A(lambda e: e.activation(out=f["eg"][:], in_=f["eg"][:], func=AF.Exp), [f["eg"]], [f["eg"]])
                                a = nq()
                                MM([lambda e, a=a: e.matmul(a[:], lhsT=wiu[0:64, ro:ro + 128], rhs=adb[0:64, :],
                                                            start=True, stop=True)], [wiu, adb], [a])
                                A(lambda e, a=a: e.activation(out=f["icl"][:], in_=a[:], func=AF.Sigmoid,
                                                              bias=pvc(L, 120 + pr), scale=1.0), [a, pv], [f["icl"]])
                                if L > 0:
                                    a = nq()
                                    MM([lambda e, a=a: e.matmul(a[:], lhsT=wvu[0:32, ro:ro + 128], rhs=vdb[0:32, :],
                                                                start=True, stop=True)], [wvu, vdb], [a])
                                    A(lambda e, a=a: e.activation(out=f["t1"][:], in_=a[:], func=AF.Sigmoid,
                                                                  bias=pvc(L, 128 + pr), scale=1.0), [a, pv], [f["t1"]])
                                    vfb = f["vf"]
                                    S.dma("sync", [(vfb[:], vfT[ro:ro + 128, t0:t0 + 512])], [R_vf], [vfb.r])
                                    V(lambda e: e.tensor_tensor(out=vfb[:], in0=vfb[:], in1=f["v"][:], op=ALU.subtract),
                                      [vfb, f["v"]], [vfb])
                                    V(lambda e: e.tensor_tensor(out=vfb[:], in0=vfb[:], in1=f["t1"][:], op=ALU.mult),
                                      [vfb, f["t1"]], [vfb])
                                    V(lambda e: e.tensor_tensor(out=f["v"][:], in0=f["v"][:], in1=vfb[:], op=ALU.add),
                                      [f["v"], vfb], [f["v"]])
                                else:
                                    S.dma("sync", [(vfT[ro:ro + 128, t0:t0 + 512], f["v"][:])], [f["v"].r], [R_vf])
                                V(lambda e: e.tensor_copy(out=VB[:], in_=f["v"][:]), [f["v"]], [VB])
                                V(lambda e: e.tensor_scalar(out=f["kkn"][:], in0=f["k"][:], scalar1=pvc(L, 136 + pr),
                                                            scalar2=None, op0=ALU.mult), [f["k"], pv], [f["kkn"]])
                                A(lambda e: e.activation(out=sqb[:], in_=f["kkn"][:], func=AF.Square), [f["kkn"]], [sqb])
                                a = nq()
                                MM([lambda e, a=a: e.matmul(a[:], lhsT=blk1, rhs=sqb[:], start=True, stop=True)],
                                   [matb, sqb], [a])
                                rsqrt_to(f["t1"], a, 1.0, 1e-12)
                                V(lambda e: e.tensor_tensor(out=f["kkn"][:], in0=f["kkn"][:], in1=f["t1"][:], op=ALU.mult),
                                  [f["kkn"], f["t1"]], [f["kkn"]])
                                V(lambda e: e.tensor_scalar(out=f["t1"][:], in0=f["icl"][:], scalar1=-1.0,
                                                            scalar2=pvc(L, 144 + pr), op0=ALU.add, op1=ALU.mult),
                                  [f["icl"], pv], [f["t1"]])
                                V(lambda e: e.scalar_tensor_tensor(out=f["k2"][:], in0=f["t1"][:], scalar=1.0, in1=f["k"][:],
                                                                   op0=ALU.add, op1=ALU.mult), [f["t1"], f["k"]], [f["k2"]])
                                V(lambda e: e.tensor_tensor(out=f["bb"][:], in0=f["kkn"][:], in1=f["icl"][:], op=ALU.mult),
                                  [f["kkn"], f["icl"]], [f["bb"]])
                                PO(lambda e, pi=pi: e.tensor_tensor(out=KT[pi][:], in0=f["k2"][:], in1=f["t3"][:], op=ALU.mult),
                                  [f["k2"], f["t3"]], [KT[pi]])
                                PO(lambda e, pi=pi: e.tensor_tensor(out=BT[pi][:], in0=f["bb"][:], in1=f["t3"][:], op=ALU.mult),
                                  [f["bb"], f["t3"]], [BT[pi]])
                                V(lambda e, pi=pi: e.scalar_tensor_tensor(out=AT[pi][:], in0=f["kkn"][:], scalar=-1.0,
                                                                          in1=f["t2"][:], op0=ALU.mult, op1=ALU.mult),
                                  [f["kkn"], f["t2"]], [AT[pi]])
                                PO(lambda e: e.tensor_tensor(out=KH[:], in0=f["k2"][:], in1=f["eg"][:], op=ALU.mult),
                                  [f["k2"], f["eg"]], [KH])
                                PO(lambda e: e.tensor_tensor(out=BH[:], in0=f["bb"][:], in1=f["eg"][:], op=ALU.mult),
                                  [f["bb"], f["eg"]], [BH])
                                V(lambda e: e.scalar_tensor_tensor(out=sqb[:], in0=f["r"][:], scalar=pvc(L, 152 + pr),
                                                                   in1=f["k2"][:], op0=ALU.mult, op1=ALU.mult),
                                  [f["r"], f["k2"], pv], [sqb])
                                a = nq()
                                MM([lambda e, a=a: e.matmul(a[:], lhsT=blk1, rhs=sqb[:], start=True, stop=True)],
                                   [matb, sqb], [a])
                                V(lambda e, a=a, pi=pi: e.tensor_tensor(out=bonus[pi][:], in0=a[:], in1=f["v"][:], op=ALU.mult),
                                  [a, f["v"]], [bonus[pi]])
                                for (srcT, dstT) in ((KH, Ktok[pi]), (BH, Btok[pi]), (VB, Vtok[pi])):
                                    MM([(lambda e, c=c, srcT=srcT: e.transpose(ptr[:, c, :], srcT[:, c * 128:(c + 1) * 128], ident))
                                        for c in range(4)], [srcT, matb], [ptr])
                                    A(lambda e, dstT=dstT: e.activation(out=dstT[:], in_=ptr[:, 0:4, :], func=AF.Copy),
                                      [ptr], [dstT])
                            lvs = lambda i: lvlm[:, i * 512:(i + 1) * 512]
                            G = 4
                            def inv_group(g0):
                                heads = list(range(g0, g0 + G))

                                def prod_k(hd, lT, rT, mask, dst):
                                    par = hd % 2
                                    ks = slice(par * 64, par * 64 + 64)
                                    a = npp()
                                    MM([(lambda e, c=c, a=a: e.matmul(
                                        a[:, c * 128:(c + 1) * 128], lhsT=lT[ks, c * 128:(c + 1) * 128],
                                        rhs=rT[ks, c * 128:(c + 1) * 128], start=True, stop=True)) for c in range(4)],
                                       [lT, rT], [a])
                                    V(lambda e, a=a: e.tensor_tensor(out=dst[:], in0=a[:], in1=mask, op=ALU.mult),
                                      [a, cst], [dst])

                                for hd in heads:
                                    pi = hd // 2
                                    prod_k(hd, AT[pi], BT[pi], mS_ts, Mh[hd - g0])
                                    prod_k(hd, BT[pi], AT[pi], mS_st, Ph[hd - g0])
                                for hd in heads:
                                    pi = hd // 2
                                    prod_k(hd, KT[pi], AT[pi], mS_st, AakT[hd])
                                    prod_k(hd, BT[pi], RT[pi], mI_st, ArbT[hd])
                                    prod_k(hd, KT[pi], RT[pi], mI_st, ArkT[hd])
                                cur = {}
                                TI = V if L == 0 else G_
                                for hd in heads:
                                    i = hd - g0
                                    Tc, TTc = Twh[i][0], TTwh[i][0]
                                    TI(lambda e, Tc=Tc, i=i: e.tensor_tensor(out=Tc[:], in0=Mh[i][:], in1=lvs(0), op=ALU.mult), [Mh[i], lvlm], [Tc])
                                    TI(lambda e, Tc=Tc: e.tensor_tensor(out=Tc[:], in0=Tc[:], in1=identx4, op=ALU.add), [Tc, cst], [Tc])
                                    TI(lambda e, TTc=TTc, i=i: e.tensor_tensor(out=TTc[:], in0=Ph[i][:], in1=lvs(1), op=ALU.mult), [Ph[i], lvlm], [TTc])
                                    TI(lambda e, TTc=TTc: e.tensor_tensor(out=TTc[:], in0=TTc[:], in1=identx4, op=ALU.add), [TTc, cst], [TTc])
                                    cur[hd] = (Tc, TTc)
                                for k in range(1, 7):
                                    last = k == 6
                                    for hd in heads:
                                        i = hd - g0
                                        Tc, TTc = cur[hd]
                                        a = npp()
                                        MM([(lambda e, c=c, a=a, TTc=TTc, i=i: e.matmul(
                                            a[:, c * 128:(c + 1) * 128], lhsT=Mh[i][:, c * 128:(c + 1) * 128],
                                            rhs=TTc[:, c * 128:(c + 1) * 128], start=True, stop=True)) for c in range(4)],
                                           [Mh[i], TTc], [a])
                                        V(lambda e, a=a, k=k, i=i: e.tensor_tensor(out=Zbh[i][1][:], in0=a[:], in1=lvs(2 * k + 1), op=ALU.mult),
                                          [a, lvlm], [Zbh[i][1]])
                                        if not last:
                                            a = npp()
                                            MM([(lambda e, c=c, a=a, Tc=Tc, i=i: e.matmul(
                                                a[:, c * 128:(c + 1) * 128], lhsT=Ph[i][:, c * 128:(c + 1) * 128],
                                                rhs=Tc[:, c * 128:(c + 1) * 128], start=True, stop=True)) for c in range(4)],
                                               [Ph[i], Tc], [a])
                                            V(lambda e, a=a, k=k, i=i: e.tensor_tensor(out=Zbh[i][0][:], in0=a[:], in1=lvs(2 * k), op=ALU.mult),
                                              [a, lvlm], [Zbh[i][0]])
                                    for hd in heads:
                                        i = hd - g0
                                        Tc, TTc = cur[hd]
                                        Tn = Twh[i][k % 2]
                                        TTn = TT[hd] if last else TTwh[i][k % 2]
                                        a3 = npp()
                                        fns = []
                                        for c in range(4):
                                            fns.append(lambda e, c=c, a3=a3, Tc=Tc, i=i: e.matmul(
                                                a3[:, c * 128:(c + 1) * 128], lhsT=Tc[:, c * 128:(c + 1) * 128],
                                                rhs=Zbh[i][1][:, c * 128:(c + 1) * 128], start=True, stop=False))
                                            fns.append(lambda e, c=c, a3=a3, TTc=TTc: e.matmul(
                                                a3[:, c * 128:(c + 1) * 128], lhsT=ident,
                                                rhs=TTc[:, c * 128:(c + 1) * 128], start=False, stop=True))
                                        MM(fns, [Tc, TTc, Zbh[i][1], matb], [a3])
                                        A(lambda e, a3=a3, TTn=TTn: e.activation(out=TTn[:], in_=a3[:], func=AF.Copy), [a3], [TTn])
                                        if not last:
                                            a3 = npp()
                                            fns = []
                                            for c in range(4):
                                                fns.append(lambda e, c=c, a3=a3, TTc=TTc, i=i: e.matmul(
                                                    a3[:, c * 128:(c + 1) * 128], lhsT=TTc[:, c * 128:(c + 1) * 128],
                                                    rhs=Zbh[i][0][:, c * 128:(c + 1) * 128], start=True, stop=False))
                                                fns.append(lambda e, c=c, a3=a3, Tc=Tc: e.matmul(
                                                    a3[:, c * 128:(c + 1) * 128], lhsT=ident,
                                                    rhs=Tc[:, c * 128:(c + 1) * 128], start=False, stop=True))
                                            MM(fns, [Tc, TTc, Zbh[i][0], matb], [a3])
                                            A(lambda e, a3=a3, Tn=Tn: e.activation(out=Tn[:], in_=a3[:], func=AF.Copy), [a3], [Tn])
                                        cur[hd] = (Tn, TTn)
                            prep_pair(0)
                            prep_pair(1)
                            S.rec = []
                            prep_pair(2)
                            prep_pair(3)
                            recA = S.rec
                            S.rec = []
                            inv_group(0)
                            recB = S.rec
                            S.rec = None
                            S.replay_merge(recA, recB)
                            inv_group(G)
                            for c in range(4):
                                cs = slice(c * 128, (c + 1) * 128)
                                yps = npp()
                                wps = npp()
                                fns = []
                                for hd in range(2 * NP):
                                    pi, par = hd // 2, hd % 2
                                    Hb = HbO if par else HbE
                                    fns.append(lambda e, hd=hd, pi=pi, Hb=Hb: e.matmul(
                                        wps[:, hd * 64:(hd + 1) * 64], lhsT=AT[pi][:, cs], rhs=Hb[:, pi, :],
                                        start=True, stop=False))
                                    fns.append(lambda e, hd=hd, pi=pi, par=par: e.matmul(
                                        wps[:, hd * 64:(hd + 1) * 64], lhsT=AakT[hd][:, cs],
                                        rhs=Vtok[pi][:, c, par * 64:par * 64 + 64], start=False, stop=True))
                                MM(fns, AT + [HbE, HbO] + AakT + Vtok, [wps])
                                V(lambda e, wps=wps: e.tensor_copy(out=Wsb[:].rearrange("p h v -> p (h v)"), in_=wps[:]),
                                  [wps], [Wsb])
                                for pi in range(NP):
                                    V(lambda e, pi=pi, c=c: e.tensor_scalar(out=Hd[:, pi, :], in0=H32[:, pi, :],
                                                                            scalar1=egC[pi][:, c:c + 1], scalar2=None, op0=ALU.mult),
                                      [H32, egC[pi]], [Hd], append=(pi > 0))
                                ups = npp()
                                MM([(lambda e, hd=hd: e.matmul(ups[:, hd * 64:(hd + 1) * 64], lhsT=TT[hd][:, cs],
                                                               rhs=Wsb[:, hd, :], start=True, stop=True))
                                    for hd in range(2 * NP)], TT + [Wsb], [ups])
                                A(lambda e, ups=ups: e.activation(out=Usb[:].rearrange("p h v -> p (h v)"), in_=ups[:],
                                                                  func=AF.Copy), [ups], [Usb])
                                fns = []
                                for hd in range(2 * NP):
                                    pi, par = hd // 2, hd % 2
                                    Hb = HbO if par else HbE
                                    o = lambda: yps[par * 64:par * 64 + 64, pi * 128:(pi + 1) * 128]
                                    fns.append(lambda e, pi=pi, par=par, Hb=Hb: e.matmul(
                                        yps[par * 64:par * 64 + 64, pi * 128:(pi + 1) * 128], lhsT=Hb[:, pi, :],
                                        rhs=RT[pi][:, cs], start=True, stop=False))
                                    fns.append(lambda e, hd=hd, pi=pi, par=par: e.matmul(
                                        yps[par * 64:par * 64 + 64, pi * 128:(pi + 1) * 128], lhsT=Usb[:, hd, :],
                                        rhs=ArbT[hd][:, cs], start=False, stop=False))
                                    fns.append(lambda e, hd=hd, pi=pi, par=par: e.matmul(
                                        yps[par * 64:par * 64 + 64, pi * 128:(pi + 1) * 128],
                                        lhsT=Vtok[pi][:, c, par * 64:par * 64 + 64],
                                        rhs=ArkT[hd][:, cs], start=False, stop=True))
                                MM(fns, [HbE, HbO, Usb] + RT + ArbT + ArkT + Vtok, [yps])
                                for pi in range(NP):
                                    A(lambda e, pi=pi: e.activation(out=Yt[pi][:, cs], in_=yps[:, pi * 128:(pi + 1) * 128],
                                                                    func=AF.Copy), [yps], [Yt[pi]])
                                hps = npp()
                                fns = []
                                for hd in range(2 * NP):
                                    pi, par = hd // 2, hd % 2
                                    fns.append(lambda e, hd=hd, pi=pi, par=par: e.matmul(
                                        hps[par * 64:par * 64 + 64, pi * 64:(pi + 1) * 64],
                                        lhsT=Btok[pi][:, c, par * 64:par * 64 + 64], rhs=Usb[:, hd, :],
                                        start=True, stop=False))
                                    fns.append(lambda e, hd=hd, pi=pi, par=par: e.matmul(
                                        hps[par * 64:par * 64 + 64, pi * 64:(pi + 1) * 64],
                                        lhsT=Ktok[pi][:, c, par * 64:par * 64 + 64],
                                        rhs=Vtok[pi][:, c, par * 64:par * 64 + 64], start=False, stop=True))
                                MM(fns, [Usb] + Btok + Ktok + Vtok, [hps])
                                V(lambda e, hps=hps: e.tensor_tensor(out=H32[:].rearrange("p h v -> p (h v)"),
                                                                     in0=Hd[:].rearrange("p h v -> p (h v)"),
                                                                     in1=hps[:, 0:NP * 64], op=ALU.add), [Hd, hps], [H32])
                                A(lambda e: e.activation(out=HbE[0:64, :, :], in_=H32[0:64, :, :], func=AF.Copy), [H32], [HbE])
                                A(lambda e: e.activation(out=HbO[64:128, :, :], in_=H32[64:128, :, :], func=AF.Copy),
                                  [H32], [HbO])
                            sq_ = [KH, BH, VB, sqb]
                            t1_ = [f["t1"], f["t2"], f["t3"], f["g"]]
                            yo_ = RT
                            PR = range(NP)
                            acc_ = {}
                            for pi in PR:
                                A(lambda e, pi=pi: e.activation(out=sq_[pi][:], in_=Yt[pi][:], func=AF.Copy), [Yt[pi]], [sq_[pi]])
                            for pi in PR:
                                a = npp()
                                acc_[pi] = a
                                MM([lambda e, a=a, pi=pi: e.matmul(a[:], lhsT=blkm, rhs=sq_[pi][:], start=True, stop=True)],
                                   [matb, sq_[pi]], [a])
                            for pi in PR:
                                V(lambda e, pi=pi, a=acc_[pi]: e.tensor_tensor(out=Yt[pi][:], in0=Yt[pi][:], in1=a[:], op=ALU.subtract),
                                  [Yt[pi], acc_[pi]], [Yt[pi]])
                            for pi in PR:
                                A(lambda e, pi=pi: e.activation(out=sq_[pi][:], in_=Yt[pi][:], func=AF.Square), [Yt[pi]], [sq_[pi]])
                            for pi in PR:
                                a = npp()
                                acc_[pi] = a
                                MM([lambda e, a=a, pi=pi: e.matmul(a[:], lhsT=blkm, rhs=sq_[pi][:], start=True, stop=True)],
                                   [matb, sq_[pi]], [a])
                            for pi in PR:
                                A(lambda e, pi=pi, a=acc_[pi]: e.activation(out=t1_[pi][:], in_=a[:], func=AF.Ln, bias=64e-5, scale=1.0),
                                  [acc_[pi]], [t1_[pi]])
                            for pi in PR:
                                A(lambda e, pi=pi: e.activation(out=t1_[pi][:], in_=t1_[pi][:], func=AF.Exp, scale=-0.5),
                                  [t1_[pi]], [t1_[pi]])
                            for pi in PR:
                                V(lambda e, pi=pi: e.tensor_tensor(out=Yt[pi][:], in0=Yt[pi][:], in1=t1_[pi][:], op=ALU.mult),
                                  [Yt[pi], t1_[pi]], [Yt[pi]])
                            for pi in PR:
                                pr = bt * NP + pi
                                V(lambda e, pi=pi, pr=pr: e.tensor_scalar(out=Yt[pi][:], in0=Yt[pi][:], scalar1=pvc(L, 160 + pr),
                                                                          scalar2=pvc(L, 168 + pr), op0=ALU.mult, op1=ALU.add),
                                  [Yt[pi], pv], [Yt[pi]])
                            for pi in PR:
                                V(lambda e, pi=pi: e.tensor_tensor(out=Yt[pi][:], in0=Yt[pi][:], in1=bonus[pi][:], op=ALU.add),
                                  [Yt[pi], bonus[pi]], [Yt[pi]])
                            for pi in PR:
                                ro = (bt * NP + pi) * 128
                                a = npp()
                                acc_[pi] = a
                                MM([lambda e, a=a, ro=ro: e.matmul(a[:], lhsT=wg0[:, ro:ro + 128], rhs=sg0[:], start=True, stop=False),
                                    lambda e, a=a, ro=ro: e.matmul(a[:], lhsT=wg1[0:32, ro:ro + 128], rhs=sg1[0:32, :],
                                                                   start=False, stop=True)], [wg0, wg1, sg0, sg1], [a])
                            for pi in PR:
                                ro = (bt * NP + pi) * 128
                                V(lambda e, pi=pi, a=acc_[pi]: e.tensor_tensor(out=yo_[pi][:], in0=Yt[pi][:], in1=a[:], op=ALU.mult),
                                  [Yt[pi], acc_[pi]], [yo_[pi]])
                                S.dma("sync", [(yT[ro:ro + 128, t0:t0 + 512], yo_[pi][:])], [yo_[pi].r], [R_yT])
                S.barrier()
                S.release(scope_res.pop(id(st), []))
            with ExitStack() as st:
                ulin2 = [sb(st, "ulin%d" % i, [128, 512], F32) for i in range(2)]
                ugat2 = [sb(st, "ugat%d" % i, [128, 512], F32) for i in range(2)]
                ulh = sb(st, "ulh", [128, 32], F32)
                ugh = sb(st, "ugh", [128, 32], F32)
                ub = sb(st, "ub", [128, 544], BF16)
                dg = sb(st, "dg", [128, 8, 31, 128], BF16)
                cT = sb(st, "cT", [128, 8, 512], F32)
                cb = sb(st, "cb", [128, 8, 512], BF16)
                tq2 = [sb(st, "tq%d" % i, [128, 512], F32) for i in range(2)]
                mean = sb(st, "mean", [128, 512], F32)
                rs = sb(st, "rs", [128, 512], F32)
                yo2 = [sb(st, "yoc%d" % i, [128, 512], BF16) for i in range(2)]
                pcv = [ps(st, "pcv%d" % i, [128, 512]) for i in range(4)]
                pst = [ps(st, "pst%d" % i, [128, 512]) for i in range(2)]
                ccnt = {"p": 0}
                for ch in range(8):
                    for j in range(31):
                        V(lambda e, j=j, ch=ch: e.tensor_scalar(out=dg[:, ch, j, :], in0=matb[:, 0:128],
                                                                scalar1=pvc(L, 200 + j * 8 + ch), scalar2=None,
                                                                op0=ALU.mult), [matb, pv], [dg], append=((ch, j) != (0, 0)))
                for b in range(NSEQ):
                    for seg in range(NSEG):
                        emit_casts(n=30)
                        t0 = b * T + seg * 512
                        halo = 30 if seg > 0 else 0
                        for ch in range(8):
                            r0 = RC + ch * 128
                            V(lambda e: e.memset(ub[:, 0:32], 0.0), [], [ub])
                            ulin, ugat = ulin2[ch % 2], ugat2[ch % 2]
                            if halo:
                                src0 = seg * 512 - halo
                                S.dma("sync", [(ulh[:, 0:halo], zT[r0:r0 + 128, b, 1 + src0:1 + src0 + halo])], [R_zT], [ulh.r])
                                S.dma("sync", [(ugh[:, 0:halo], zT[r0 + DC:r0 + DC + 128, b, 1 + src0:1 + src0 + halo])], [R_zT], [ugh.r])
                                A(lambda e: e.activation(out=ugh[:, 0:30], in_=ugh[:, 0:30], func=AF.Sigmoid), [ugh], [ugh])
                                V(lambda e: e.tensor_tensor(out=ub[:, 2:32], in0=ulh[:, 0:30], in1=ugh[:, 0:30], op=ALU.mult), [ulh, ugh], [ub])
                            src0 = seg * 512
                            S.dma("sync", [(ulin[:], zT[r0:r0 + 128, b, 1 + src0:1 + src0 + 512])], [R_zT], [ulin.r])
                            S.dma("sync", [(ugat[:], zT[r0 + DC:r0 + DC + 128, b, 1 + src0:1 + src0 + 512])], [R_zT], [ugat.r])
                            A(lambda e, ugat=ugat: e.activation(out=ugat[:], in_=ugat[:], func=AF.Sigmoid), [ugat], [ugat])
                            V(lambda e, ulin=ulin, ugat=ugat: e.tensor_tensor(out=ub[:, 32:544], in0=ulin[:], in1=ugat[:], op=ALU.mult),
                              [ulin, ugat], [ub])
                            a = pcv[ccnt["p"] % 4]
                            ccnt["p"] += 1
                            MM([(lambda e, j=j, a=a, ch=ch: e.matmul(a[:], lhsT=dg[:, ch, j, :], rhs=ub[:, 2 + j:2 + j + 512],
                                                              start=(j == 0), stop=(j == 30))) for j in range(31)],
                               [dg, ub], [a])
                            A(lambda e, a=a, ch=ch: e.activation(out=cT[:, ch, :], in_=a[:], func=AF.Identity,
                                                                 bias=pvc(L, 176 + ch), scale=1.0), [a, pv], [cT])
                        A(lambda e: e.activation(out=cb[:], in_=cT[:], func=AF.Copy), [cT], [cb])
                        MM([(lambda e, ch=ch: e.matmul(pst[0][:], lhsT=onesC, rhs=cb[:, ch, :], start=(ch == 0), stop=(ch == 7)))
                            for ch in range(8)], [cb, matb], [pst[0]])
                        V(lambda e: e.tensor_copy(out=mean[:], in_=pst[0][:]), [pst[0]], [mean])
                        for ch in range(8):
                            V(lambda e, ch=ch: e.tensor_tensor(out=cT[:, ch, :], in0=cT[:, ch, :], in1=mean[:], op=ALU.subtract),
                              [cT, mean], [cT])
                        A(lambda e: e.activation(out=cb[:], in_=cT[:], func=AF.Square), [cT], [cb])
                        MM([(lambda e, ch=ch: e.matmul(pst[1][:], lhsT=onesC, rhs=cb[:, ch, :], start=(ch == 0), stop=(ch == 7)))
                            for ch in range(8)], [cb, matb], [pst[1]])
                        A(lambda e: e.activation(out=rs[:], in_=pst[1][:], func=AF.Ln, bias=1e-5, scale=1.0), [pst[1]], [rs])
                        A(lambda e: e.activation(out=rs[:], in_=rs[:], func=AF.Exp, scale=-0.5), [rs], [rs])
                        for ch in range(8):
                            tq, yo = tq2[ch % 2], yo2[ch % 2]
                            V(lambda e, ch=ch, tq=tq: e.tensor_tensor(out=tq[:], in0=cT[:, ch, :], in1=rs[:], op=ALU.mult), [cT, rs], [tq])
                            V(lambda e, ch=ch, tq=tq: e.tensor_scalar(out=tq[:], in0=tq[:], scalar1=pvc(L, 184 + ch),
                                                                      scalar2=pvc(L, 192 + ch), op0=ALU.mult, op1=ALU.add), [tq, pv], [tq])
                            A(lambda e, tq=tq, yo=yo: e.activation(out=yo[:], in_=tq[:], func=AF.Silu), [tq], [yo])
                            S.dma("sync", [(yT[DR + ch * 128:DR + (ch + 1) * 128, t0:t0 + 512], yo[:])], [yo.r], [R_yT])
                S.barrier()
                S.release(scope_res.pop(id(st), []))

        for L in range(DEPTH + 1):
            tok_stage(L)
            if L < DEPTH:
                seq_stage(L)
        S.barrier()
        build.ninst = S.ninst
        build.nsem = S.nsem
    return nc


def _col(v, n):
    return np.ascontiguousarray(np.asarray(v, np.float32).reshape(n, 128).T)


def _pack_pv(inp, DEPTH):
    pv = np.zeros((DEPTH, 128, NPV), np.float32)
    for l in range(DEPTH):
        P = pv[l]
        for i, nm in enumerate(("norm_mix_pre", "norm_mix_post", "norm_mlp_pre", "norm_mlp_post", "norm_ple")):
            P[:, 16 * i:16 * i + 16] = _col(inp[nm][l], 16)
        mu = np.asarray(inp["mu_shift"][l], np.float32)
        for j in range(3):
            P[:, 80 + 8 * j:88 + 8 * j] = _col(mu[j * DR:(j + 1) * DR], 8)
        P[0:64, 104] = mu[3072:3136]
        P[0:64, 105] = mu[3136:3200]
        P[0:128, 106] = mu[3200:3328]
        P[0:32, 107] = mu[3328:3360]
        if l > 0:
            P[0:32, 108] = np.asarray(inp["mu_shift_vmix"][l - 1], np.float32)
            P[:, 128:136] = _col(inp["v0"][l - 1], 8)
        for i, nm in enumerate(("w0", "a0", None, "k_k", "k_a", "r_k", "gn_gain", "gn_bias")):
            if nm is not None:
                P[:, 112 + 8 * i:120 + 8 * i] = _col(np.asarray(inp[nm][l]).reshape(-1), 8)
        for i, nm in enumerate(("dw_b", "conv_ln_gain", "conv_ln_bias")):
            P[:, 176 + 8 * i:184 + 8 * i] = _col(inp[nm][l], 8)
        dw = np.asarray(inp["dw_w"][l], np.float32)
        for j in range(31):
            P[:, 200 + 8 * j:208 + 8 * j] = _col(dw[j], 8)
    return pv


def _consts():
    c = np.zeros((128, NCST), np.float32)
    c[:, 0:128] = np.eye(128)
    c[:, 128:256] = 1.0 / 2048
    c[0:64, 256:320] = 1.0
    c[64:128, 320:384] = 1.0
    c[:, 384:512] = 1.0 / 1024
    c[0:64, 512:576] = 1.0 / 64
    c[64:128, 576:640] = 1.0 / 64
    i = np.arange(128)
    st_s = (i[:, None] < i[None, :]).astype(np.float32)
    st_i = (i[:, None] <= i[None, :]).astype(np.float32)
    ts_s = (i[None, :] < i[:, None]).astype(np.float32)
    c[:, 640:1152] = np.tile(st_s, (1, 4))
    c[:, 1152:1664] = np.tile(st_i, (1, 4))
    c[:, 1664:2176] = np.tile(ts_s, (1, 4))
    c[:, 2176:2304] = 1.0
    c[:, 2304:2816] = np.tile(np.eye(128, dtype=np.float32), (1, 4))
    return c


def _levels():
    i = np.arange(128)
    out = np.zeros((128, 14 * 512), np.float32)
    for k in range(7):
        b = 1 << k
        t, s_ = i[:, None], i[None, :]
        off = ((t // (2 * b)) == (s_ // (2 * b))) & ((t % (2 * b)) >= b) & ((s_ % (2 * b)) < b)
        off = off.astype(np.float32)
        out[:, (2 * k) * 512:(2 * k + 1) * 512] = np.tile(off, (1, 4))
        out[:, (2 * k + 1) * 512:(2 * k + 2) * 512] = np.tile(off.T, (1, 4))
    return out


_CACHE = {}


def run(inputs, DEPTH, NSEQ, T, ncores):
    key = (DEPTH, NSEQ, T)
    if key not in _CACHE:
        _CACHE[key] = build(DEPTH, NSEQ, T)
    nc = _CACHE[key]
    f32 = lambda a: np.ascontiguousarray(np.asarray(a, np.float32))
    x = f32(inputs["x"])
    p = f32(inputs["p"])
    LV = max(DEPTH - 1, 1)
    shared = {
        "w_in": f32(inputs["w_in"]), "w_out": f32(inputs["w_out"]), "w_up": f32(inputs["w_up"]),
        "w_down": f32(inputs["w_down"]), "w_ple": f32(inputs["w_ple"]), "w_ple_gate": f32(inputs["w_ple_gate"]),
        "w_decay_up": f32(inputs["w_decay_up"]), "w_iclr_up": f32(inputs["w_iclr_up"]),
        "w_gate_up": f32(inputs["w_gate_up"]),
        "pv": _pack_pv(inputs, DEPTH), "cst": _consts(), "lvl": _levels(),
    }
    wv = np.zeros((LV, D, 32), np.float32)
    wu = np.zeros((LV, 32, DR), np.float32)
    if DEPTH > 1:
        wv[:] = f32(inputs["w_in_vmix"])[:DEPTH - 1]
        wu[:] = f32(inputs["w_vmix_up"])[:DEPTH - 1]
    shared["w_in_vmix"] = wv
    shared["w_vmix_up"] = wu
    in_maps = []
    for c in range(ncores):
        xs = x[c * NSEQ:(c + 1) * NSEQ].reshape(NSEQ * T, D)
        ps_ = p[:, c * NSEQ:(c + 1) * NSEQ].reshape(DEPTH, NSEQ * T, DPLE)
        m = dict(shared)
        m["xT"] = np.ascontiguousarray(xs.T)
        m["pT"] = np.ascontiguousarray(ps_.transpose(0, 2, 1))
        in_maps.append(m)
    res = run_bass_kernel_spmd(nc, in_maps, core_ids=list(range(ncores)))
    outs = [np.asarray(r["outT"]).T.reshape(NSEQ, T, D) for r in res.results]
    return np.ascontiguousarray(np.concatenate(outs, axis=0).astype(np.float32))


def kernel(**inputs):
    return run(inputs, 4, 2, 2048, 8)
```
